# Optimizing a Trainium2 kernel written in Bass

```python
import jax, jax.numpy as jnp
from jax import lax
import numpy as np

D_MODEL = 1024
BATCH = 8
SEQ = 2048
DEPTH = 4

N_MIXERS = 2
CONV_WIDTH = 3
ML_HEADS = 4
ML_V_DIM = D_MODEL // ML_HEADS
ML_QK_DIM = ML_V_DIM // 2
ML_QK_W = ML_HEADS * ML_QK_DIM
ML_V_W = ML_HEADS * ML_V_DIM
ML_IN_W = 2 * ML_QK_W + 2 * ML_V_W + 2 * ML_HEADS
ML_CHUNK = 64
GATE_SOFTCAP = 15.0
D_FF = ((8 * D_MODEL // 3 + 127) // 128) * 128
EPS = 1e-6

kernel_name = "hybrid_shortconv_mlstm_convffn_adaln"


def rms_norm(x, g):
    xf = x.astype(jnp.float32)
    y = xf * lax.rsqrt(jnp.mean(xf * xf, axis=-1, keepdims=True) + EPS)
    return (y * g.astype(jnp.float32)).astype(x.dtype)


def modulate(h, shift, scale):
    return h * (1 + scale[:, None, :]) + shift[:, None, :]


def causal_dwconv(x, w):
    k_w = w.shape[0]
    s = x.shape[1]
    xp = jnp.pad(x, ((0, 0), (k_w - 1, 0), (0, 0)))
    y = w[0] * xp[:, 0:s]
    for j in range(1, k_w):
        y = y + w[j] * xp[:, j:j + s]
    return y


def softcap(t):
    return GATE_SOFTCAP * jnp.tanh(t / GATE_SOFTCAP)


def short_conv_mixer(h, w_in, conv_w, w_out):
    bcu = h @ w_in
    gb, gc, u = jnp.split(bcu, 3, axis=-1)
    y = gb * causal_dwconv(gc * u, conv_w)
    return y @ w_out


def mlstm_chunkwise(q, k, v, ig, logf):
    bsz, nh, s, dk = q.shape
    dv = v.shape[-1]
    nc = s // ML_CHUNK

    def to_chunks(t):
        t = t.reshape((bsz, nh, nc, ML_CHUNK) + t.shape[3:])
        return jnp.moveaxis(t, 2, 0)

    causal = jnp.tril(jnp.ones((ML_CHUNK, ML_CHUNK), dtype=bool))

    def step(carry, inp):
        c_st, n_st, m_st = carry
        qc, kc, vc, ic, fc = inp
        b = jnp.cumsum(fc, axis=-1)
        d_log = b[..., :, None] - b[..., None, :] + ic[..., None, :]
        d_log = jnp.where(causal, d_log, -jnp.inf)
        inter = b + m_st[..., None]
        m_t = jnp.maximum(inter, jnp.max(d_log, axis=-1))
        p = jnp.exp(d_log - m_t[..., None])
        sc = jnp.einsum('bhtd,bhsd->bhts', qc, kc) * p
        a = jnp.exp(inter - m_t)
        num = a[..., None] * jnp.einsum('bhvd,bhtd->bhtv', c_st, qc) + jnp.einsum('bhts,bhsv->bhtv', sc, vc)
        den = a * jnp.einsum('bhd,bhtd->bht', n_st, qc) + jnp.sum(sc, axis=-1)
        hc = num / jnp.maximum(jnp.abs(den), jnp.exp(-m_t))[..., None]
        b_last = b[..., -1]
        w_log = b_last[..., None] - b + ic
        m_new = jnp.maximum(b_last + m_st, jnp.max(w_log, axis=-1))
        decay = jnp.exp(b_last + m_st - m_new)
        w = jnp.exp(w_log - m_new[..., None])
        c_new = decay[..., None, None] * c_st + jnp.einsum('bhs,bhsv,bhsd->bhvd', w, vc, kc)
        n_new = decay[..., None] * n_st + jnp.einsum('bhs,bhsd->bhd', w, kc)
        return (c_new, n_new, m_new), hc

    init = (jnp.zeros((bsz, nh, dv, dk), jnp.float32),
            jnp.zeros((bsz, nh, dk), jnp.float32),
            jnp.zeros((bsz, nh), jnp.float32))
    _, hs = lax.scan(step, init, (to_chunks(q), to_chunks(k), to_chunks(v), to_chunks(ig), to_chunks(logf)))
    return jnp.moveaxis(hs, 0, 2).reshape(bsz, nh, s, dv)


def mlstm_mixer(h, w_in, b_i, b_f, norm_g, w_out):
    bsz, s, _ = h.shape
    proj = h @ w_in
    q, k, v, o, ig, fg = jnp.split(
        proj, [ML_QK_W, 2 * ML_QK_W, 2 * ML_QK_W + ML_V_W, 2 * ML_QK_W + 2 * ML_V_W,
               2 * ML_QK_W + 2 * ML_V_W + ML_HEADS], axis=-1)

    def heads(t, d):
        return t.reshape(bsz, s, ML_HEADS, d).transpose(0, 2, 1, 3).astype(jnp.float32)

    qh = heads(q, ML_QK_DIM) * (ML_QK_DIM ** -0.5)
    kh = heads(k, ML_QK_DIM)
    vh = heads(v, ML_V_DIM)
    i_pre = softcap((ig + b_i).astype(jnp.float32)).transpose(0, 2, 1)
    logf = jax.nn.log_sigmoid(softcap((fg + b_f).astype(jnp.float32))).transpose(0, 2, 1)
    hh = mlstm_chunkwise(qh, kh, vh, i_pre, logf)
    hh = hh * lax.rsqrt(jnp.mean(hh * hh, axis=-1, keepdims=True) + EPS)
    hh = hh.transpose(0, 2, 1, 3).reshape(bsz, s, ML_V_W) * norm_g.astype(jnp.float32)
    y = (hh * jax.nn.sigmoid(o.astype(jnp.float32))).astype(h.dtype)
    return y @ w_out


def conv_ffn(h, w_up, conv_w, conv_b, w_down):
    u = causal_dwconv(h @ w_up, conv_w) + conv_b
    g, val = jnp.split(u, 2, axis=-1)
    return (jax.nn.silu(g) * val) @ w_down


def setup_inputs(seed: int = 0) -> dict:
    key = jax.random.key(seed)
    ks = jax.random.split(key, 24)
    n_sc = (DEPTH + 1) // 2
    n_ml = DEPTH // 2
    d = D_MODEL
    f32 = jnp.float32
    nrm = lambda k, shape, s: jax.random.normal(k, shape, f32) * s
    return {
        "x": nrm(ks[0], (BATCH, SEQ, d), 1.0),
        "c": nrm(ks[1], (BATCH, d), 1.0),
        "ada_w": nrm(ks[2], (DEPTH, d, 6 * d), 0.2 * d ** -0.5),
        "ada_b": nrm(ks[3], (DEPTH, 6 * d), 0.02),
        "norm_mix_g": 1.0 + nrm(ks[4], (DEPTH, d), 0.05),
        "norm_ffn_g": 1.0 + nrm(ks[5], (DEPTH, d), 0.05),
        "sc_w_in": nrm(ks[6], (n_sc, d, 3 * d), d ** -0.5),
        "sc_conv_w": nrm(ks[7], (n_sc, CONV_WIDTH, d), CONV_WIDTH ** -0.5),
        "sc_w_out": nrm(ks[8], (n_sc, d, d), d ** -0.5),
        "ml_w_in": nrm(ks[9], (n_ml, d, ML_IN_W), d ** -0.5),
        "ml_b_i": nrm(ks[10], (n_ml, ML_HEADS), 0.1),
        "ml_b_f": jnp.linspace(3.0, 6.0, ML_HEADS, dtype=f32)[None, :] + nrm(ks[11], (n_ml, ML_HEADS), 0.1),
        "ml_norm_g": 1.0 + nrm(ks[12], (n_ml, ML_V_W), 0.05),
        "ml_w_out": nrm(ks[13], (n_ml, ML_V_W, d), ML_V_W ** -0.5),
        "ffn_w_up": nrm(ks[14], (DEPTH, d, 2 * D_FF), d ** -0.5),
        "ffn_conv_w": nrm(ks[15], (DEPTH, CONV_WIDTH, 2 * D_FF), CONV_WIDTH ** -0.5),
        "ffn_conv_b": nrm(ks[16], (DEPTH, 2 * D_FF), 0.02),
        "ffn_w_down": nrm(ks[17], (DEPTH, D_FF, d), D_FF ** -0.5),
        "final_norm_g": 1.0 + nrm(ks[18], (d,), 0.05),
    }


def reference(x, c, ada_w, ada_b, norm_mix_g, norm_ffn_g, sc_w_in, sc_conv_w, sc_w_out,
              ml_w_in, ml_b_i, ml_b_f, ml_norm_g, ml_w_out, ffn_w_up, ffn_conv_w, ffn_conv_b,
              ffn_w_down, final_norm_g):
    cond = jax.nn.silu(c)
    for layer in range(DEPTH):
        mod = cond @ ada_w[layer] + ada_b[layer]
        sh1, sc1, g1, sh2, sc2, g2 = jnp.split(mod, 6, axis=-1)
        h = modulate(rms_norm(x, norm_mix_g[layer]), sh1, sc1)
        j = layer // N_MIXERS
        if layer % N_MIXERS == 0:
            y = short_conv_mixer(h, sc_w_in[j], sc_conv_w[j], sc_w_out[j])
        else:
            y = mlstm_mixer(h, ml_w_in[j], ml_b_i[j], ml_b_f[j], ml_norm_g[j], ml_w_out[j])
        x = x + (1 + g1)[:, None, :] * y
        h = modulate(rms_norm(x, norm_ffn_g[layer]), sh2, sc2)
        x = x + (1 + g2)[:, None, :] * conv_ffn(h, ffn_w_up[layer], ffn_conv_w[layer], ffn_conv_b[layer], ffn_w_down[layer])
    return rms_norm(x, final_norm_g)
```

```python
import numpy as np
from contextlib import ExitStack
import concourse.bass as bass
import concourse.mybir as mybir
from concourse.bass_utils import run_bass_kernel_spmd

F32 = mybir.dt.float32
BF16 = mybir.dt.bfloat16
AF = mybir.ActivationFunctionType
ALU = mybir.AluOpType
DT_SIZE = {F32: 4, BF16: 2}

D = 1024
T = 2048
DEPTH = 4
DFF = 2816
NUP = 2 * DFF // 128
NCH = 8
HALF = 1024
TT = 512
NS = 4
EPS = 1e-6
HEADS = 4
DV = 256
DK = 128
VA = DV + 1
KVW = 512 + HEADS * VA
ML_IN = 3080

PL = 320
P_ADAB, P_NMG, P_NFG, P_MIX, P_FCW, P_FCB = 0, 48, 56, 64, 136, 268
P_C = DEPTH * PL
P_FNG = P_C + 8
NP_COLS = P_FNG + 8


def _isap(v):
    return hasattr(v, "tensor") and hasattr(v, "ap")


def _is_dram(ap):
    return "DRAM" in str(ap.space).upper()


def _is_psum(ap):
    return "PSUM" in str(ap.space).upper()


def ap_ranges(ap):
    dsz = DT_SIZE[ap.dtype]
    dims = [list(d) for d in ap.ap]
    pstride = dims[0][0]
    off = ap.offset % pstride if pstride else ap.offset
    free = dims[1:]
    if not free:
        return [(off * dsz, (off + 1) * dsz)]
    ls, lc = free[-1]
    span = (lc - 1) * abs(ls) + 1
    outer = free[:-1]
    n_outer = 1
    for s, c in outer:
        n_outer *= c
    offs = [0]
    if n_outer > 256:
        hi = sum((c - 1) * abs(s) for s, c in free) + 1
        return [(off * dsz, (off + hi) * dsz)]
    for s, c in outer:
        offs = [o + i * s for o in offs for i in range(c)]
    ivs = sorted((off + o, off + o + span) for o in offs)
    out = []
    for a, b in ivs:
        if out and a <= out[-1][1]:
            out[-1][1] = max(out[-1][1], b)
        else:
            out.append([a, b])
    return [(a * dsz, b * dsz) for a, b in out]


class Sched:
    ENGS = ("pe", "dve", "act", "pool", "sp")

    def __init__(self, nc, es):
        self.nc = nc
        self.es = es
        self.semobj = {e: es.enter_context(nc.semaphore("s_" + e)) for e in self.ENGS}
        self.cnt = {e: 0 for e in self.ENGS}
        self.ops = {e: [] for e in self.ENGS}
        self.seen = {e: {} for e in self.ENGS}
        self.mem = {}
        self.dmacnt = {}
        self.nops = 0

    def dma_sem(self, name):
        self.semobj[name] = self.es.enter_context(self.nc.semaphore(name))
        self.dmacnt[name] = 0
        return name

    def _access(self, name, lo, hi, write, tok, deps):
        ivs = self.mem.get(name)
        if ivs is None:
            ivs = [[0, 1 << 40, None, {}]]
        new = []
        for iv in ivs:
            a, b, w, r = iv
            if b <= lo or a >= hi:
                new.append(iv)
                continue
            if a < lo:
                new.append([a, lo, w, dict(r)])
                a = lo
            tail = None
            if b > hi:
                tail = [hi, b, w, dict(r)]
                b = hi
            if w is not None and w != tok:
                if deps.get(w[0], 0) < w[1]:
                    deps[w[0]] = w[1]
            if write:
                for k, v in r.items():
                    if (k, v) != tok and deps.get(k, 0) < v:
                        deps[k] = v
                new.append([a, b, tok, {}])
            else:
                r2 = dict(r)
                if r2.get(tok[0], 0) < tok[1]:
                    r2[tok[0]] = tok[1]
                new.append([a, b, w, r2])
            if tail:
                new.append(tail)
        merged = []
        for iv in new:
            if merged and merged[-1][1] == iv[0] and merged[-1][2] == iv[2] and merged[-1][3] == iv[3]:
                merged[-1][1] = iv[1]
            else:
                merged.append(iv)
        self.mem[name] = merged

    def op(self, eng, fn, reads=(), writes=(), mark=True, dma=None, extra_deps=()):
        if dma is not None:
            self.dmacnt[dma] += 16
            tok = (dma, self.dmacnt[dma])
        elif mark:
            self.cnt[eng] += 1
            tok = (eng, self.cnt[eng])
        else:
            tok = (eng, self.cnt[eng] + 1)
        deps = {}
        for k, v in extra_deps:
            if deps.get(k, 0) < v:
                deps[k] = v
        pdeps = {}
        for ap in list(reads) + list(writes):
            if _is_psum(ap):
                for lo, hi in ap_ranges(ap):
                    self._access(ap.tensor.name, lo // 2048 * 2048, (hi + 2047) // 2048 * 2048, True, tok, pdeps)
        for k, v in pdeps.items():
            if k != eng and deps.get(k, 0) < v:
                deps[k] = v
        for ap in reads:
            if not _is_dram(ap) and not _is_psum(ap):
                for lo, hi in ap_ranges(ap):
                    self._access(ap.tensor.name, lo, hi, False, tok, deps)
        for ap in writes:
            if not _is_dram(ap) and not _is_psum(ap):
                for lo, hi in ap_ranges(ap):
                    self._access(ap.tensor.name, lo, hi, True, tok, deps)
        waits = []
        for k, v in deps.items():
            if (k, v) == tok:
                continue
            if k == eng and eng == "pe":
                continue
            if k == eng and v > self.cnt[eng] - (1 if (mark and dma is None) else 0):
                continue
            if self.seen[eng].get(k, 0) >= v:
                continue
            self.seen[eng][k] = v
            waits.append((k, v))
        self.ops[eng].append((waits, fn, tok if (mark or dma is not None) else None))
        self.nops += 1
        return tok

    def wait_only(self, eng, toks):
        waits = []
        for k, v in toks:
            if self.seen[eng].get(k, 0) < v:
                self.seen[eng][k] = v
                waits.append((k, v))
        self.ops[eng].append((waits, None, None))

    def act(self, out, in_, func, **kw):
        reads = [in_] + [v for k, v in kw.items() if _isap(v) and k != "accum_out"]
        writes = [out] + ([kw["accum_out"]] if "accum_out" in kw else [])
        return self.op("act", lambda e: e.activation(out=out, in_=in_, func=func, **kw), reads, writes)

    def tt(self, out, in0, in1, op):
        return self.op("dve", lambda e: e.tensor_tensor(out=out, in0=in0, in1=in1, op=op), [in0, in1], [out])

    def tt_pool(self, out, in0, in1, op):
        return self.op("pool", lambda e: e.tensor_tensor(out=out, in0=in0, in1=in1, op=op), [in0, in1], [out])

    def copy_pool(self, out, in_):
        return self.op("pool", lambda e: e.tensor_copy(out=out, in_=in_), [in_], [out])

    def stt(self, out, in0, scalar, in1, op0, op1):
        reads = [in0, in1] + ([scalar] if _isap(scalar) else [])
        return self.op("dve", lambda e: e.scalar_tensor_tensor(out=out, in0=in0, scalar=scalar, in1=in1, op0=op0, op1=op1),
                       reads, [out])

    def ts(self, out, in0, s1, s2, op0, op1=None):
        reads = [in0] + [s for s in (s1, s2) if _isap(s)]
        if op1 is None:
            return self.op("dve", lambda e: e.tensor_scalar(out=out, in0=in0, scalar1=s1, scalar2=None, op0=op0), reads, [out])
        return self.op("dve", lambda e: e.tensor_scalar(out=out, in0=in0, scalar1=s1, scalar2=s2, op0=op0, op1=op1), reads, [out])

    def copy(self, out, in_):
        return self.op("dve", lambda e: e.tensor_copy(out=out, in_=in_), [in_], [out])

    def memset(self, ap, val, writes=None):
        return self.op("dve", lambda e: e.memset(ap, val), [], [ap] if writes is None else writes)

    def recip(self, out, in_):
        return self.op("dve", lambda e: e.reciprocal(out=out, in_=in_), [in_], [out])

    def mm(self, out, lhsT, rhs, start, stop, mark):
        return self.op("pe", lambda e: e.matmul(out, lhsT=lhsT, rhs=rhs, start=start, stop=stop),
                       [lhsT, rhs], [out], mark=mark)

    def group(self, out, lhs_list, rhs_list):
        n = len(lhs_list)
        for k in range(n):
            self.mm(out, lhs_list[k], rhs_list[k], k == 0, k == n - 1, k == n - 1)

    def transpose(self, out, in_, ident):
        return self.op("pe", lambda e: e.transpose(out, in_, ident), [in_, ident], [out])

    def dma(self, eng, out, in_, sem, writes=None):
        w = writes if writes is not None else ([] if _is_dram(out) else [out])
        r = [] if _is_dram(in_) else [in_]
        return self.op(eng, lambda e: e.dma_start(out=out, in_=in_), r, w, dma=sem)

    def emit(self):
        nc = self.nc
        me = self

        def mk(engname):
            def body(e):
                for waits, fn, tok in me.ops[engname]:
                    for k, v in waits:
                        e.wait_ge(me.semobj[k], v)
                    if fn is None:
                        continue
                    ins = fn(e)
                    if tok is not None:
                        ins.then_inc(me.semobj[tok[0]], 16 if tok[0] in me.dmacnt else 1)
            return body

        self.stats = {e: len(v) for e, v in self.ops.items()}
        with nc.Block() as block:
            block.tensor(mk("pe"))
            block.vector(mk("dve"))
            block.scalar(mk("act"))
            block.gpsimd(mk("pool"))
            block.sync(mk("sp"))


def build_program(layer_ids, final_norm=True):
    nl = len(layer_ids)
    sc_layers = [l for l in layer_ids if l % 2 == 0]
    ml_layers = [l for l in layer_ids if l % 2 == 1]
    nc = bass.Bass("TRN2", target_bir_lowering=False)
    dr = {}
    dr["xT"] = nc.dram_tensor("xT", [D, T], F32, kind="ExternalInput").ap()
    dr["params"] = nc.dram_tensor("params", [128, NP_COLS], F32, kind="ExternalInput").ap()
    dr["consts"] = nc.dram_tensor("consts", [128, 512], F32, kind="ExternalInput").ap()
    dr["ada_w"] = nc.dram_tensor("ada_w", [nl, D, 6 * D], F32, kind="ExternalInput").ap()
    dr["ffn_w_up"] = nc.dram_tensor("ffn_w_up", [nl, D, 2 * DFF], F32, kind="ExternalInput").ap()
    dr["ffn_w_down"] = nc.dram_tensor("ffn_w_down", [nl, DFF, D], F32, kind="ExternalInput").ap()
    if sc_layers:
        dr["sc_w_in"] = nc.dram_tensor("sc_w_in", [len(sc_layers), D, 3 * D], F32, kind="ExternalInput").ap()
        dr["sc_w_out"] = nc.dram_tensor("sc_w_out", [len(sc_layers), D, D], F32, kind="ExternalInput").ap()
    if ml_layers:
        dr["ml_w_in"] = nc.dram_tensor("ml_w_in", [len(ml_layers), D, ML_IN], F32, kind="ExternalInput").ap()
        dr["ml_w_out"] = nc.dram_tensor("ml_w_out", [len(ml_layers), D, D], F32, kind="ExternalInput").ap()
    yT = nc.dram_tensor("yT", [D, T], F32, kind="ExternalOutput").ap()

    with ExitStack() as es:
        S = Sched(nc, es)

        def sb(name, shape, dt):
            return es.enter_context(nc.sbuf_tensor(name, shape, dt))
        x_sb = sb("x_sb", [128, NCH, T], F32)
        h_sb = sb("h_sb", [128, NCH, HALF], BF16)
        act_sb = sb("act_sb", [128, 22 * HALF], BF16)
        ybuf = sb("ybuf", [128, 8192], BF16)
        tmpU = sb("tmpU", [128, 2 * 4 * 1026], BF16)
        ring = sb("ring", [128, NS, NCH, 512], BF16)
        params = sb("params_sb", [128, NP_COLS], F32)
        consts = sb("consts_sb", [128, 512], F32)
        ident_bf = sb("ident_bf", [128, 128], BF16)
        ones_bf = sb("ones_bf", [128, 128], BF16)
        tri_bf = sb("tri_bf", [128, 128], BF16)
        mods = sb("mods", [128, 2, 48], F32)
        condT = sb("condT", [128, NCH], BF16)
        halo_pr = sb("halo_pr", [128, NCH, 2], BF16)
        halo_u = sb("halo_u", [128, NUP, 2], BF16)
        C32 = sb("C32", [128, HEADS, VA], F32)
        Cbf = sb("Cbf", [128, HEADS, VA], BF16)
        ps = es.enter_context(nc.psum_tensor("ps", [128, 8, 512], F32))

        ident_f = consts[:, 0:128]
        tri_f = consts[:, 128:256]
        posmask_f = consts[:, 256:384]
        ones_f = consts[:, 384:512]

        def view(t, boff, shape, dt):
            n = 1
            for s in shape:
                n *= s
            nb = n * DT_SIZE[dt]
            a = t[:, boff // 2:(boff + nb) // 2]
            if dt != BF16:
                a = a.bitcast(dt)
            if len(shape) == 1:
                return a
            names = " ".join("d%d" % i for i in range(len(shape)))
            kw = {"d%d" % i: shape[i] for i in range(len(shape) - 1)}
            return a.rearrange("p (%s) -> p %s" % (names, names), **kw)

        actv = view(act_sb, 0, [22, HALF], BF16)
        U = [view(tmpU, i * 8208, [4, 1026], BF16) for i in range(2)]
        rs_v = view(ybuf, 0, [2, 512], F32)
        tmpn_v = view(ybuf, 4096, [2, 512], F32)
        accv = view(ybuf, 0, [4, 1024], BF16)
        accg = view(ybuf, 8192, [4, 1024], BF16)
        yb_sc = view(ybuf, 0, [NCH, HALF], BF16)
        yb_ml = view(ybuf, 0, [8, 8, 128], BF16)
        sc_gc = view(act_sb, 0, [4, 1024], BF16)
        sc_pr = view(act_sb, 8192, [4, 1026], BF16)
        sc_cv = view(act_sb, 16400, [4, 1024], BF16)
        kv = view(act_sb, 0, [8, KVW], BF16)
        kvv = kv[:, :, 512:].rearrange("p c (h v) -> p c h v", h=HEADS)
        so_v = view(act_sb, 8 * KVW * 2, [2, 512], BF16)
        o_ = [0]

        def carve(shape, dt):
            n = 1
            for s in shape:
                n *= s
            nb = (n * DT_SIZE[dt] + 3) // 4 * 4
            v = view(tmpU, o_[0], shape, dt)
            o_[0] += nb
            return v
        G1 = carve([64], F32)
        TH = carve([64], F32)
        IGt = carve([8, 4], F32)
        Lp = carve([8, 4], F32)
        E4 = carve([8, 4], F32)
        X16 = carve([2, 16], F32)
        EX = carve([8, 16], F32)
        AT = carve([2, HEADS, 128], BF16)
        KW = carve([2, HEADS, 128], BF16)
        HH = carve([4, HEADS, DV], BF16)
        DN = carve([8, 4], F32)
        RR = carve([8, 4], F32)
        SSQ = carve([8, 4], F32)
        SQ = carve([8, 4], F32)
        JUNK = carve([DV], BF16)
        NUM = view(act_sb, 8 * KVW * 2 + 2048, [8, HEADS, VA], BF16)
        assert o_[0] <= 2 * 8208, o_[0]

        ring_sem = [S.dma_sem("ring%d" % i) for i in range(NS)]
        ld_sem = [S.dma_sem("ld%d" % i) for i in range(NCH)]
        misc_sem = S.dma_sem("ldmisc")
        misc2_sem = S.dma_sem("ldmisc2")
        st_sem = S.dma_sem("st")

        S.dma("sp", params[:, :], dr["params"], misc_sem)
        S.dma("sp", consts[:, :], dr["consts"], misc2_sem)
        xTv = dr["xT"].rearrange("(c p) t -> p c t", p=128)
        for c in range(NCH):
            S.dma("sp", x_sb[:, c, :], xTv[:, c, :], ld_sem[c])
        S.copy(ident_bf[:, :], ident_f)
        S.copy(ones_bf[:, :], ones_f)
        S.copy(tri_bf[:, :], tri_f)
        S.act(condT[:, :], params[:, P_C:P_C + 8], AF.Silu)

        blocks = []
        state = {"issued": 0, "consumed": 0}

        def issue_next():
            i = state["issued"]
            src, kc, ncols = blocks[i]
            s = i % NS
            S.dma("pool", ring[:, s, 0:kc, 0:ncols], src, ring_sem[s], writes=[ring[:, s, :, :]])
            state["issued"] += 1

        def wblock(w_ap, r0, nrows, c0, ncols):
            src = w_ap[r0:r0 + nrows, c0:c0 + ncols].rearrange("(kc p) n -> p kc n", p=128)
            blocks.append((src, nrows // 128, ncols))
            return len(blocks) - 1

        def use_block(bi):
            assert bi == state["consumed"], (bi, state["consumed"])
            while state["issued"] < min(len(blocks), bi + NS):
                issue_next()
            state["consumed"] += 1
            return ring[:, bi % NS, :, :]

        def done_block(bi):
            while state["issued"] < min(len(blocks), bi + 1 + NS):
                issue_next()

        bank_rr = [0]

        BANKS = (0, 1, 2, 3, 5, 6, 7)

        def next_bank():
            b = BANKS[bank_rr[0]]
            bank_rr[0] = (bank_rr[0] + 1) % len(BANKS)
            return ps[:, b, :]

        def pcol(col):
            return params[:, col:col + 1]

        def wchunks(slot, m, nk=NCH):
            return [slot[:, k, m * 128:(m + 1) * 128] for k in range(nk)]

        def hcols(sl):
            return [h_sb[:, k, sl] for k in range(NCH)]

        def ada_parts(li):
            l = layer_ids[li]
            slot_l = l % 2
            pb = l * PL
            psa = ps[:, 4, 448:496]

            def reg(j):
                return wblock(dr["ada_w"][li], 0, D, j * 512, 512)

            def consume(j, bi):
                slot = use_block(bi)
                for m in range(4):
                    q = 4 * j + m
                    S.group(psa[:, q:q + 1], wchunks(slot, m), [condT[:, k:k + 1] for k in range(NCH)])
                done_block(bi)

            def finalize():
                md = mods[:, slot_l, :]
                S.tt(md, psa, params[:, pb + P_ADAB:pb + P_ADAB + 48], ALU.add)
                for (o_sc, o_g, pg) in ((8, 16, P_NMG), (32, 40, P_NFG)):
                    a_sc = mods[:, slot_l, o_sc:o_sc + 8]
                    a_g = mods[:, slot_l, o_g:o_g + 8]
                    S.stt(a_sc, a_sc, 1.0, params[:, pb + pg:pb + pg + 8], ALU.add, ALU.mult)
                    S.ts(a_g, a_g, 1.0, None, ALU.add)
            return reg, consume, finalize

        def ada_phase(li):
            reg, consume, finalize = ada_parts(li)
            bis = [reg(j) for j in range(12)]

            def run():
                for j, bi in enumerate(bis):
                    consume(j, bi)
                finalize()
            return run

        def rstd_tile(rsv, tok_cols, sq_dst_fn):
            for c in range(NCH):
                S.act(sq_dst_fn(c), x_sb[:, c, tok_cols], AF.Square)
            pn = next_bank()
            S.group(pn, [ones_bf[:, :]] * NCH, [sq_dst_fn(c) for c in range(NCH)])
            S.act(rsv, pn, AF.Sqrt, scale=1.0 / D, bias=eps_col)
            S.recip(rsv, rsv)

        def norm_steps(hf, gm_ap, sh_ap):
            steps = []
            for t2 in range(2):
                tok = slice(hf * HALF + t2 * TT, hf * HALF + (t2 + 1) * TT)
                hs = slice(t2 * TT, (t2 + 1) * TT)
                rsv = rs_v[:, t2, :]

                def s1(tok=tok, hs=hs):
                    for c in range(NCH):
                        S.act(h_sb[:, c, hs], x_sb[:, c, tok], AF.Square)

                def s2(hs=hs, rsv=rsv):
                    pn = next_bank()
                    S.group(pn, [ones_bf[:, :]] * NCH, [h_sb[:, c, hs] for c in range(NCH)])
                    S.act(rsv, pn, AF.Sqrt, scale=1.0 / D, bias=eps_col)
                    S.recip(rsv, rsv)

                def s3(tok=tok, hs=hs, rsv=rsv):
                    for c in range(NCH):
                        tn = tmpn_v[:, c % 2, :]
                        S.stt(tn, x_sb[:, c, tok], gm_ap[:, c:c + 1], rsv, ALU.mult, ALU.mult)
                        S.act(h_sb[:, c, hs], tn, AF.Identity, bias=sh_ap[:, c:c + 1], scale=1.0)
                steps += [s1, s2, s3]
            return steps

        def norm_phase(hf, gm_ap, sh_ap):
            for st in norm_steps(hf, gm_ap, sh_ap):
                st()

        def xupdate(pb_ap, mc, hf, t2, gate_ap):
            tok = slice(hf * HALF + t2 * TT, hf * HALF + (t2 + 1) * TT)
            xs = x_sb[:, mc, tok]
            S.stt(xs, pb_ap, gate_ap[:, mc:mc + 1], xs, ALU.mult, ALU.add)

        def conv3(dst, src, o, w_cols, bias_col):
            w0, w1, w2 = w_cols
            if bias_col is None:
                S.ts(dst, src[:, o + 2:o + 2 + TT], w2, None, ALU.mult)
            else:
                S.ts(dst, src[:, o + 2:o + 2 + TT], w2, bias_col, ALU.mult, ALU.add)
            S.stt(dst, src[:, o + 1:o + 1 + TT], w1, dst, ALU.mult, ALU.add)
            S.stt(dst, src[:, o:o + TT], w0, dst, ALU.mult, ALU.add)

        def halo_in(dst2, halo_ap, hf):
            if hf == 0:
                S.memset(dst2, 0.0)
            else:
                S.copy(dst2, halo_ap)

        def outproj(wo, hf, md, rhs_fn):
            gate = md[:, 16:24]
            for cg, bi in enumerate(wo):
                slot = use_block(bi)
                for m in range(4):
                    mc = 4 * cg + m
                    for t2 in range(2):
                        pb_ap = next_bank()
                        S.group(pb_ap, wchunks(slot, m), [rhs_fn(k, t2) for k in range(NCH)])
                        xupdate(pb_ap, mc, hf, t2, gate)
                done_block(bi)

        def shortconv_phase(li, hf, md):
            l = layer_ids[li]
            j = sc_layers.index(l)
            pb = l * PL
            w_in = dr["sc_w_in"][j]
            seq = []
            for i in range(2):
                seq.append(("gc", i, wblock(w_in, 0, D, D + 512 * i, 512)))
                seq.append(("u", i, wblock(w_in, 0, D, 2 * D + 512 * i, 512)))
                seq.append(("gb", i, wblock(w_in, 0, D, 512 * i, 512)))
            wo = [wblock(dr["sc_w_out"][j], 0, D, 512 * cg, 512) for cg in range(2)]

            def run():
                for kind, i, bi in seq:
                    slot = use_block(bi)
                    for m in range(4):
                        f = 4 * i + m
                        if kind == "u":
                            halo_in(sc_pr[:, m, 0:2], halo_pr[:, f, :], hf)
                        for t2 in range(2):
                            hs = slice(t2 * TT, (t2 + 1) * TT)
                            pb_ap = next_bank()
                            S.group(pb_ap, wchunks(slot, m), hcols(hs))
                            if kind == "gc":
                                S.act(sc_gc[:, m, hs], pb_ap, AF.Copy)
                            elif kind == "u":
                                S.tt(sc_pr[:, m, 2 + t2 * TT:2 + (t2 + 1) * TT], pb_ap, sc_gc[:, m, hs], ALU.mult)
                                wc = [pcol(pb + P_MIX + jj * 8 + f) for jj in range(3)]
                                conv3(sc_cv[:, m, hs], sc_pr[:, m, :], t2 * TT, wc, None)
                                if t2 == 1:
                                    S.copy(halo_pr[:, f, :], sc_pr[:, m, 1024:1026])
                            else:
                                S.tt(yb_sc[:, f, hs], pb_ap, sc_cv[:, m, hs], ALU.mult)
                    done_block(bi)
                outproj(wo, hf, md, lambda k, t2: yb_sc[:, k, t2 * TT:(t2 + 1) * TT])
            return run

        def ffn_phase(li, hf, md, ada_li=None, next_norm=None):
            l = layer_ids[li]
            pb = l * PL
            w_up = dr["ffn_w_up"][li]
            w_dn = dr["ffn_w_down"][li]
            seq = []
            ada = ada_parts(ada_li[0]) if ada_li is not None else None
            for i in range(6):
                ncols = 512 if i < 5 else 256
                seq.append(("v", i, ncols, wblock(w_up, 0, D, DFF + 512 * i, ncols), None, None))
                aj = ada_li[1] + i if ada else None
                seq.append(("g", i, ncols, wblock(w_up, 0, D, 512 * i, ncols), aj, ada[0](aj) if ada else None))
            dseq = []
            for cg in range(2):
                for kb in range(3):
                    nk = 8 if kb < 2 else 6
                    dseq.append((cg, kb, nk, wblock(w_dn, kb * 1024, nk * 128, cg * 512, 512)))

            def run():
                for si, (kind, i, ncols, bi, aj, abi) in enumerate(seq):
                    slot = use_block(bi)
                    Ub = U[0] if kind == "v" else U[1]
                    for m in range(ncols // 128):
                        jj = 4 * i + m
                        q = jj + (22 if kind == "v" else 0)
                        halo_in(Ub[:, m, 0:2], halo_u[:, q, :], hf)
                        w0, w1, w2 = [pcol(pb + P_FCW + t * NUP + q) for t in range(3)]
                        bc = pcol(pb + P_FCB + q)
                        accs = []
                        for t2 in range(2):
                            hs = slice(t2 * TT, (t2 + 1) * TT)
                            pb_ap = next_bank()
                            S.group(pb_ap, wchunks(slot, m), hcols(hs))
                            S.act(Ub[:, m, 2 + t2 * TT:2 + (t2 + 1) * TT], pb_ap, AF.Copy)
                            acc = (accv if kind == "v" else accg)[:, m, hs]
                            if kind == "v":
                                S.act(acc, pb_ap, AF.Identity, scale=w2, bias=bc)
                            accs.append(acc)
                        if kind == "g":
                            for t2 in range(2):
                                o = t2 * TT
                                S.ts(accs[t2], Ub[:, m, o + 2:o + 2 + TT], w2, bc, ALU.mult, ALU.add)
                        for t2 in range(2):
                            o = t2 * TT
                            S.stt(accs[t2], Ub[:, m, o + 1:o + 1 + TT], w1, accs[t2], ALU.mult, ALU.add)
                        for t2 in range(2):
                            o = t2 * TT
                            S.stt(accs[t2], Ub[:, m, o:o + TT], w0, accs[t2], ALU.mult, ALU.add)
                        S.copy_pool(halo_u[:, q, :], Ub[:, m, 1024:1026])
                        if kind == "g":
                            for t2 in range(2):
                                S.act(accs[t2], accs[t2], AF.Silu)
                            for t2 in range(2):
                                hs = slice(t2 * TT, (t2 + 1) * TT)
                                S.tt_pool(actv[:, jj, hs], accs[t2], accv[:, m, hs], ALU.mult)
                    done_block(bi)
                    if abi is not None:
                        ada[1](aj, abi)
                if ada and ada_li[2]:
                    ada[2]()
                gate = md[:, 40:48]
                side = norm_steps(*next_norm) if next_norm is not None else []
                for di, (cg, kb, nk, bi) in enumerate(dseq):
                    slot = use_block(bi)
                    for m in range(4):
                        mc = 4 * cg + m
                        for t2 in range(2):
                            hs = slice(t2 * TT, (t2 + 1) * TT)
                            pb_ap = next_bank()
                            S.group(pb_ap, wchunks(slot, m, nk), [actv[:, kb * 8 + k, hs] for k in range(nk)])
                            xupdate(pb_ap, mc, hf, t2, gate)
                    done_block(bi)
                    if di < len(side):
                        side[di]()
            return run

        def mlstm_phase(li, hf, md):
            l = layer_ids[li]
            j = ml_layers.index(l)
            pb = l * PL
            w_in = dr["ml_w_in"][j]
            b_g = wblock(w_in, 0, D, 3072, 8)
            b_q = wblock(w_in, 0, D, 0, 512)
            b_k = wblock(w_in, 0, D, 512, 512)
            b_v = [wblock(w_in, 0, D, 1024 + 512 * i, 512) for i in range(2)]
            b_o = [wblock(w_in, 0, D, 2048 + 512 * i, 512) for i in range(2)]
            wo = [wblock(dr["ml_w_out"][j], 0, D, 512 * cg, 512) for cg in range(2)]

            def run():
                if hf == 0:
                    S.memset(C32[:, :, :], 0.0)
                    S.memset(Cbf[:, :, :], 0.0)
                S.memset(kvv[:, :, :, DV:VA], 1.0, writes=[kv[:, :, 512:]])
                slot = use_block(b_g)
                pg = ps[:, 4, 384:448]
                for c8 in range(8):
                    S.group(pg[:, c8 * 8:(c8 + 1) * 8], hcols(slice(c8 * 128, (c8 + 1) * 128)),
                            [slot[:, k, 0:8] for k in range(NCH)])
                done_block(b_g)
                S.tt(G1, pg, params[:, pb + P_MIX + 8:pb + P_MIX + 72], ALU.add)
                S.act(TH, G1, AF.Tanh, scale=1.0 / 15.0)
                th3 = TH.rearrange("p (c g) -> p c g", c=8)
                S.ts(IGt, th3[:, :, 0:4], 15.0, None, ALU.mult)
                S.act(E4, th3[:, :, 4:8], AF.Exp, scale=-15.0)
                S.act(Lp, E4, AF.Ln, bias=one_col, scale=1.0)
                for which, bi in (("q", b_q), ("k", b_k)):
                    slot = use_block(bi)
                    for m in range(HEADS):
                        for t2 in range(2):
                            hs = slice(t2 * TT, (t2 + 1) * TT)
                            pb_ap = next_bank()
                            S.group(pb_ap, wchunks(slot, m), hcols(hs))
                            d_ = yb_ml[:, t2 * 4:(t2 + 1) * 4, (m if which == "q" else 4 + m), :]
                            src = pb_ap.rearrange("p (a b) -> p a b", a=4)
                            S.act(d_, src, AF.Copy, scale=(DK ** -0.5 if which == "q" else 1.0))
                    if which == "k":
                        for c8 in range(8):
                            pb_ap = next_bank()
                            S.group(pb_ap, hcols(slice(c8 * 128, (c8 + 1) * 128)), [slot[:, k, 0:512] for k in range(NCH)])
                            S.act(kv[:, c8, 0:512], pb_ap, AF.Copy)
                    done_block(bi)
                for i, bi in enumerate(b_v):
                    slot = use_block(bi)
                    for c8 in range(8):
                        pb_ap = next_bank()
                        S.group(pb_ap, hcols(slice(c8 * 128, (c8 + 1) * 128)), [slot[:, k, 0:512] for k in range(NCH)])
                        S.act(kvv[:, c8, 2 * i:2 * i + 2, 0:DV], pb_ap.rearrange("p (a b) -> p a b", a=2), AF.Copy)
                    done_block(bi)
                pbn = ps[:, 4, 0:8]

                def stage_a(c8):
                    par = c8 % 2
                    x16 = X16[:, par, :]
                    ex = EX[:, c8, :]
                    S.mm(pbn[:, 0:4], tri_f, Lp[:, c8, :], True, True, True)
                    S.mm(pbn[:, 4:8], ones_f, Lp[:, c8, :], True, True, True)
                    S.tt(x16[:, 0:4], IGt[:, c8, :], pbn[:, 0:4], ALU.add)
                    S.copy(x16[:, 8:12], pbn[:, 0:4])
                    S.ts(x16[:, 12:16], pbn[:, 4:8], -1.0, None, ALU.mult)
                    S.tt(x16[:, 4:8], x16[:, 0:4], pbn[:, 4:8], ALU.subtract)
                    S.act(ex, x16, AF.Exp)
                    pS = ps[:, 5 if par == 0 else 7, :].rearrange("p (h t) -> p h t", h=HEADS)
                    for h in range(HEADS):
                        S.mm(pS[:, h, :], yb_ml[:, c8, 4 + h, :], yb_ml[:, c8, h, :], True, True, True)
                    for h in range(HEADS):
                        S.stt(AT[:, par, h, :], pS[:, h, :], ex[:, h:h + 1], tri_bf[:, :], ALU.mult, ALU.mult)
                    for h in range(HEADS):
                        S.ts(KW[:, par, h, :], kv[:, c8, h * 128:(h + 1) * 128], ex[:, 4 + h:5 + h], None, ALU.mult)
                    for h in range(HEADS):
                        pn_ = ps[:, h, 0:VA]
                        S.mm(pn_, AT[:, par, h, :], kvv[:, c8, h, :], True, False, False)
                        S.mm(pn_, yb_ml[:, c8, h, :], Cbf[:, h, :], False, True, True)
                    for h in range(HEADS):
                        S.act(NUM[:, c8, h, :], ps[:, h, 0:VA], AF.Copy)
                    for h in range(HEADS):
                        S.mm(ps[:, h, 0:VA], KW[:, par, h, :], kvv[:, c8, h, :], True, True, True)
                    for h in range(HEADS):
                        S.stt(C32[:, h, :], C32[:, h, :], ex[:, 12 + h:13 + h], ps[:, h, 0:VA], ALU.mult, ALU.add)
                    for h in range(HEADS):
                        S.act(Cbf[:, h, :], C32[:, h, :], AF.Copy)

                for c8 in range(8):
                    stage_a(c8)

                S.act(DN, NUM[:, :, :, DV], AF.Abs)
                S.tt(DN, DN, EX[:, :, 8:12], ALU.max)
                S.recip(DN, DN)
                for c8 in range(8):
                    for h in range(HEADS):
                        S.act(JUNK, NUM[:, c8, h, 0:DV], AF.Square, scale=DN[:, c8, h:h + 1], accum_out=SSQ[:, c8, h:h + 1])
                S.act(SQ, SSQ, AF.Ln, scale=1.0 / DV, bias=eps_col)
                S.act(SQ, SQ, AF.Exp, scale=-0.5)
                S.tt(RR, SQ, DN, ALU.mult)
                for c8 in range(8):
                    hb = c8 % 4
                    for h in range(HEADS):
                        S.ts(HH[:, hb, h, :], NUM[:, c8, h, 0:DV], RR[:, c8, h:h + 1], None, ALU.mult)
                    pTb = ps[:, 6 if c8 % 2 == 0 else 5, :].bitcast(BF16).rearrange("p (h j t) -> p h j t", h=HEADS, j=2)
                    for h in range(HEADS):
                        for jj in range(2):
                            S.transpose(pTb[:, h, jj, :], HH[:, hb, h, jj * 128:(jj + 1) * 128], ident_bf[:, :])
                    for h in range(HEADS):
                        S.act(yb_ml[:, c8, 2 * h, :], pTb[:, h, 0, :], AF.Copy, scale=pcol(pb + P_MIX + 2 * h))
                        S.ts(yb_ml[:, c8, 2 * h + 1, :], pTb[:, h, 1, :], pcol(pb + P_MIX + 2 * h + 1), None, ALU.mult)
                for i, bi in enumerate(b_o):
                    slot = use_block(bi)
                    for m in range(4):
                        f = 4 * i + m
                        for t2 in range(2):
                            hs = slice(t2 * TT, (t2 + 1) * TT)
                            pb_ap = next_bank()
                            S.group(pb_ap, wchunks(slot, m), hcols(hs))
                            so = so_v[:, t2, :]
                            S.act(so, pb_ap, AF.Sigmoid)
                            d_ = yb_ml[:, t2 * 4:(t2 + 1) * 4, f, :]
                            S.tt(d_, d_, so.rearrange("p (a b) -> p a b", a=4), ALU.mult)
                    done_block(bi)
                outproj(wo, hf, md, lambda k, t2: yb_ml[:, t2 * 4:(t2 + 1) * 4, k, :])
            return run

        eps_col = sb("eps_col", [128, 1], F32)
        one_col = sb("one_col", [128, 1], F32)
        S.memset(eps_col[:, :], EPS)
        S.memset(one_col[:, :], 1.0)
        eps_col = eps_col[:, :]
        one_col = one_col[:, :]

        sequence = [ada_phase(0)]
        for li, l in enumerate(layer_ids):
            md = mods[:, l % 2, :]
            for hf in range(2):
                if li == 0 and hf == 0:
                    sequence.append(("norm", hf, md[:, 8:16], md[:, 0:8]))
                if l % 2 == 0:
                    sequence.append(shortconv_phase(li, hf, md))
                else:
                    sequence.append(mlstm_phase(li, hf, md))
                sequence.append(("norm", hf, md[:, 32:40], md[:, 24:32]))
                if hf == 0:
                    nn = (1, md[:, 8:16], md[:, 0:8])
                elif li + 1 < nl:
                    md2 = mods[:, layer_ids[li + 1] % 2, :]
                    nn = (0, md2[:, 8:16], md2[:, 0:8])
                else:
                    nn = None
                sequence.append(ffn_phase(li, hf, md, ada_li=((li + 1, 6 * hf, hf == 1) if li + 1 < nl else None), next_norm=nn))
        for item in sequence:
            if isinstance(item, tuple):
                norm_phase(item[1], item[2], item[3])
            else:
                item()

        yTv = yT.rearrange("(c p) t -> p c t", p=128)
        if final_norm:
            fg = params[:, P_FNG:P_FNG + 8]
            for tt in range(4):
                tok = slice(tt * TT, (tt + 1) * TT)
                rsv = rs_v[:, tt % 2, :]
                rstd_tile(rsv, tok, lambda c: h_sb[:, c, 0:TT])
                for c in range(NCH):
                    xs = x_sb[:, c, tok]
                    S.stt(xs, xs, fg[:, c:c + 1], rsv, ALU.mult, ALU.mult)
        tok_last = None
        for c in range(NCH):
            tok_last = S.dma("sp", yTv[:, c, :], x_sb[:, c, :], st_sem)
        S.wait_only("sp", [tok_last])
        S.emit()
        nc._sched_stats = S.stats
    return nc


def _consts():
    c = np.zeros((128, 512), np.float32)
    c[:, 0:128] = np.eye(128, dtype=np.float32)
    j = np.arange(128)[:, None]
    t = np.arange(128)[None, :]
    c[:, 128:256] = (j <= t).astype(np.float32)
    c[:, 256:384] = np.where(j <= t, 0.0, 30000.0)
    c[:, 384:512] = 1.0
    return c


def _chunked(v):
    v = np.asarray(v, np.float32)
    return np.ascontiguousarray(v.reshape(-1, 128).T)


def _params_for_core(b, inp):
    P = np.zeros((128, NP_COLS), np.float32)
    for l in range(DEPTH):
        pb = l * PL
        j = l // 2
        P[:, pb + P_ADAB:pb + P_ADAB + 48] = _chunked(inp["ada_b"][l])
        P[:, pb + P_NMG:pb + P_NMG + 8] = _chunked(inp["norm_mix_g"][l])
        P[:, pb + P_NFG:pb + P_NFG + 8] = _chunked(inp["norm_ffn_g"][l])
        if l % 2 == 0:
            for t in range(3):
                P[:, pb + P_MIX + t * 8:pb + P_MIX + (t + 1) * 8] = _chunked(inp["sc_conv_w"][j][t])
        else:
            P[:, pb + P_MIX:pb + P_MIX + 8] = _chunked(inp["ml_norm_g"][j])
            bif = np.concatenate([np.asarray(inp["ml_b_i"][j], np.float32), np.asarray(inp["ml_b_f"][j], np.float32)])
            P[:, pb + P_MIX + 8:pb + P_MIX + 72] = np.tile(bif, 8)[None, :]
        for t in range(3):
            P[:, pb + P_FCW + t * NUP:pb + P_FCW + (t + 1) * NUP] = _chunked(inp["ffn_conv_w"][l][t])
        P[:, pb + P_FCB:pb + P_FCB + NUP] = _chunked(inp["ffn_conv_b"][l])
    P[:, P_C:P_C + 8] = _chunked(inp["c"][b])
    P[:, P_FNG:P_FNG + 8] = _chunked(inp["final_norm_g"])
    return P


_PROG_CACHE = {}


def _get_prog(layer_ids, final_norm):
    key = (tuple(layer_ids), final_norm)
    if key not in _PROG_CACHE:
        _PROG_CACHE[key] = build_program(list(layer_ids), final_norm)
    return _PROG_CACHE[key]


def _launch(layer_ids, final_norm, xT_list, inp, params_list, consts):
    nc = _get_prog(layer_ids, final_norm)
    l0, l1 = layer_ids[0], layer_ids[-1] + 1
    sc = [l // 2 for l in layer_ids if l % 2 == 0]
    ml = [l // 2 for l in layer_ids if l % 2 == 1]
    shared = {
        "consts": consts,
        "ada_w": np.ascontiguousarray(inp["ada_w"][l0:l1]),
        "ffn_w_up": np.ascontiguousarray(inp["ffn_w_up"][l0:l1]),
        "ffn_w_down": np.ascontiguousarray(inp["ffn_w_down"][l0:l1]),
    }
    if sc:
        shared["sc_w_in"] = np.ascontiguousarray(inp["sc_w_in"][sc[0]:sc[-1] + 1])
        shared["sc_w_out"] = np.ascontiguousarray(inp["sc_w_out"][sc[0]:sc[-1] + 1])
    if ml:
        shared["ml_w_in"] = np.ascontiguousarray(inp["ml_w_in"][ml[0]:ml[-1] + 1])
        shared["ml_w_out"] = np.ascontiguousarray(inp["ml_w_out"][ml[0]:ml[-1] + 1])
    in_maps = []
    for b in range(8):
        m = dict(shared)
        m["xT"] = xT_list[b]
        m["params"] = params_list[b]
        in_maps.append(m)
    res = run_bass_kernel_spmd(nc, in_maps, core_ids=list(range(8)))
    return [np.asarray(r["yT"]) for r in res.results]


LAUNCH_GROUPS = [[0, 1, 2, 3]]


def kernel(**inputs):
    inp = {k: np.asarray(v) for k, v in inputs.items()}
    x = inp["x"].astype(np.float32, copy=False)
    consts = _consts()
    params_list = [_params_for_core(b, inp) for b in range(8)]
    xT_list = [np.ascontiguousarray(x[b].T) for b in range(8)]
    for gi, grp in enumerate(LAUNCH_GROUPS):
        xT_list = _launch(grp, gi == len(LAUNCH_GROUPS) - 1, xT_list, inp, params_list, consts)
    out = np.stack([np.ascontiguousarray(xT_list[b].T) for b in range(8)], axis=0)
    return out.astype(np.float32, copy=False)
```

```python
import numpy as np
from contextlib import ExitStack
import concourse.bass as bass
import concourse.mybir as mybir
from concourse.bass_utils import run_bass_kernel_spmd

F32 = mybir.dt.float32
BF16 = mybir.dt.bfloat16
AF = mybir.ActivationFunctionType
ALU = mybir.AluOpType
DT_SIZE = {F32: 4, BF16: 2}

D = 1024
T = 2048
DEPTH = 4
DFF = 2816
NUP = 2 * DFF // 128
NCH = 8
HALF = 1024
TT = 512
NS = 4
EPS = 1e-6
HEADS = 4
DV = 256
DK = 128
VA = DV + 1
KVW = 512 + HEADS * VA
ML_IN = 3080

PL = 320
P_ADAB, P_NMG, P_NFG, P_MIX, P_FCW, P_FCB = 0, 48, 56, 64, 136, 268
P_C = DEPTH * PL
P_FNG = P_C + 8
NP_COLS = P_FNG + 8


def _isap(v):
    return hasattr(v, "tensor") and hasattr(v, "ap")


def _is_dram(ap):
    return "DRAM" in str(ap.space).upper()


def _is_psum(ap):
    return "PSUM" in str(ap.space).upper()


def ap_ranges(ap):
    dsz = DT_SIZE[ap.dtype]
    dims = [list(d) for d in ap.ap]
    pstride = dims[0][0]
    off = ap.offset % pstride if pstride else ap.offset
    free = dims[1:]
    if not free:
        return [(off * dsz, (off + 1) * dsz)]
    ls, lc = free[-1]
    span = (lc - 1) * abs(ls) + 1
    outer = free[:-1]
    n_outer = 1
    for s, c in outer:
        n_outer *= c
    offs = [0]
    if n_outer > 256:
        hi = sum((c - 1) * abs(s) for s, c in free) + 1
        return [(off * dsz, (off + hi) * dsz)]
    for s, c in outer:
        offs = [o + i * s for o in offs for i in range(c)]
    ivs = sorted((off + o, off + o + span) for o in offs)
    out = []
    for a, b in ivs:
        if out and a <= out[-1][1]:
            out[-1][1] = max(out[-1][1], b)
        else:
            out.append([a, b])
    return [(a * dsz, b * dsz) for a, b in out]


class Sched:
    ENGS = ("pe", "dve", "act", "pool", "sp")

    def __init__(self, nc, es):
        self.nc = nc
        self.es = es
        self.semobj = {e: es.enter_context(nc.semaphore("s_" + e)) for e in self.ENGS}
        self.cnt = {e: 0 for e in self.ENGS}
        self.ops = {e: [] for e in self.ENGS}
        self.seen = {e: {} for e in self.ENGS}
        self.mem = {}
        self.dmacnt = {}
        self.nops = 0

    def dma_sem(self, name):
        self.semobj[name] = self.es.enter_context(self.nc.semaphore(name))
        self.dmacnt[name] = 0
        return name

    def _access(self, name, lo, hi, write, tok, deps):
        ivs = self.mem.get(name)
        if ivs is None:
            ivs = [[0, 1 << 40, None, {}]]
        new = []
        for iv in ivs:
            a, b, w, r = iv
            if b <= lo or a >= hi:
                new.append(iv)
                continue
            if a < lo:
                new.append([a, lo, w, dict(r)])
                a = lo
            tail = None
            if b > hi:
                tail = [hi, b, w, dict(r)]
                b = hi
            if w is not None and w != tok:
                if deps.get(w[0], 0) < w[1]:
                    deps[w[0]] = w[1]
            if write:
                for k, v in r.items():
                    if (k, v) != tok and deps.get(k, 0) < v:
                        deps[k] = v
                new.append([a, b, tok, {}])
            else:
                r2 = dict(r)
                if r2.get(tok[0], 0) < tok[1]:
                    r2[tok[0]] = tok[1]
                new.append([a, b, w, r2])
            if tail:
                new.append(tail)
        merged = []
        for iv in new:
            if merged and merged[-1][1] == iv[0] and merged[-1][2] == iv[2] and merged[-1][3] == iv[3]:
                merged[-1][1] = iv[1]
            else:
                merged.append(iv)
        self.mem[name] = merged

    def op(self, eng, fn, reads=(), writes=(), mark=True, dma=None, extra_deps=()):
        if dma is not None:
            self.dmacnt[dma] += 16
            tok = (dma, self.dmacnt[dma])
        elif mark:
            self.cnt[eng] += 1
            tok = (eng, self.cnt[eng])
        else:
            tok = (eng, self.cnt[eng] + 1)
        deps = {}
        for k, v in extra_deps:
            if deps.get(k, 0) < v:
                deps[k] = v
        pdeps = {}
        for ap in list(reads) + list(writes):
            if _is_psum(ap):
                for lo, hi in ap_ranges(ap):
                    self._access(ap.tensor.name, lo // 2048 * 2048, (hi + 2047) // 2048 * 2048, True, tok, pdeps)
        for k, v in pdeps.items():
            if k != eng and deps.get(k, 0) < v:
                deps[k] = v
        for ap in reads:
            if not _is_dram(ap) and not _is_psum(ap):
                for lo, hi in ap_ranges(ap):
                    self._access(ap.tensor.name, lo, hi, False, tok, deps)
        for ap in writes:
            if not _is_dram(ap) and not _is_psum(ap):
                for lo, hi in ap_ranges(ap):
                    self._access(ap.tensor.name, lo, hi, True, tok, deps)
        waits = []
        for k, v in deps.items():
            if (k, v) == tok:
                continue
            if k == eng and eng == "pe":
                continue
            if k == eng and v > self.cnt[eng] - (1 if (mark and dma is None) else 0):
                continue
            if self.seen[eng].get(k, 0) >= v:
                continue
            self.seen[eng][k] = v
            waits.append((k, v))
        self.ops[eng].append((waits, fn, tok if (mark or dma is not None) else None))
        self.nops += 1
        return tok

    def wait_only(self, eng, toks):
        waits = []
        for k, v in toks:
            if self.seen[eng].get(k, 0) < v:
                self.seen[eng][k] = v
                waits.append((k, v))
        self.ops[eng].append((waits, None, None))

    def act(self, out, in_, func, **kw):
        reads = [in_] + [v for k, v in kw.items() if _isap(v) and k != "accum_out"]
        writes = [out] + ([kw["accum_out"]] if "accum_out" in kw else [])
        return self.op("act", lambda e: e.activation(out=out, in_=in_, func=func, **kw), reads, writes)

    def tt(self, out, in0, in1, op):
        return self.op("dve", lambda e: e.tensor_tensor(out=out, in0=in0, in1=in1, op=op), [in0, in1], [out])

    def tt_pool(self, out, in0, in1, op):
        return self.op("pool", lambda e: e.tensor_tensor(out=out, in0=in0, in1=in1, op=op), [in0, in1], [out])

    def copy_pool(self, out, in_):
        return self.op("pool", lambda e: e.tensor_copy(out=out, in_=in_), [in_], [out])

    def stt(self, out, in0, scalar, in1, op0, op1):
        reads = [in0, in1] + ([scalar] if _isap(scalar) else [])
        return self.op("dve", lambda e: e.scalar_tensor_tensor(out=out, in0=in0, scalar=scalar, in1=in1, op0=op0, op1=op1),
                       reads, [out])

    def ts(self, out, in0, s1, s2, op0, op1=None):
        reads = [in0] + [s for s in (s1, s2) if _isap(s)]
        if op1 is None:
            return self.op("dve", lambda e: e.tensor_scalar(out=out, in0=in0, scalar1=s1, scalar2=None, op0=op0), reads, [out])
        return self.op("dve", lambda e: e.tensor_scalar(out=out, in0=in0, scalar1=s1, scalar2=s2, op0=op0, op1=op1), reads, [out])

    def copy(self, out, in_):
        return self.op("dve", lambda e: e.tensor_copy(out=out, in_=in_), [in_], [out])

    def memset(self, ap, val, writes=None):
        return self.op("dve", lambda e: e.memset(ap, val), [], [ap] if writes is None else writes)

    def recip(self, out, in_):
        return self.op("dve", lambda e: e.reciprocal(out=out, in_=in_), [in_], [out])

    def mm(self, out, lhsT, rhs, start, stop, mark):
        return self.op("pe", lambda e: e.matmul(out, lhsT=lhsT, rhs=rhs, start=start, stop=stop),
                       [lhsT, rhs], [out], mark=mark)

    def group(self, out, lhs_list, rhs_list):
        n = len(lhs_list)
        for k in range(n):
            self.mm(out, lhs_list[k], rhs_list[k], k == 0, k == n - 1, k == n - 1)

    def transpose(self, out, in_, ident):
        return self.op("pe", lambda e: e.transpose(out, in_, ident), [in_, ident], [out])

    def dma(self, eng, out, in_, sem, writes=None):
        w = writes if writes is not None else ([] if _is_dram(out) else [out])
        r = [] if _is_dram(in_) else [in_]
        return self.op(eng, lambda e: e.dma_start(out=out, in_=in_), r, w, dma=sem)

    def emit(self):
        nc = self.nc
        me = self

        def mk(engname):
            def body(e):
                for waits, fn, tok in me.ops[engname]:
                    for k, v in waits:
                        e.wait_ge(me.semobj[k], v)
                    if fn is None:
                        continue
                    ins = fn(e)
                    if tok is not None:
                        ins.then_inc(me.semobj[tok[0]], 16 if tok[0] in me.dmacnt else 1)
            return body

        self.stats = {e: len(v) for e, v in self.ops.items()}
        with nc.Block() as block:
            block.tensor(mk("pe"))
            block.vector(mk("dve"))
            block.scalar(mk("act"))
            block.gpsimd(mk("pool"))
            block.sync(mk("sp"))


def build_program(layer_ids, final_norm=True):
    nl = len(layer_ids)
    sc_layers = [l for l in layer_ids if l % 2 == 0]
    ml_layers = [l for l in layer_ids if l % 2 == 1]
    nc = bass.Bass("TRN2", target_bir_lowering=False)
    dr = {}
    dr["xT"] = nc.dram_tensor("xT", [D, T], F32, kind="ExternalInput").ap()
    dr["params"] = nc.dram_tensor("params", [128, NP_COLS], F32, kind="ExternalInput").ap()
    dr["consts"] = nc.dram_tensor("consts", [128, 512], F32, kind="ExternalInput").ap()
    dr["ada_w"] = nc.dram_tensor("ada_w", [nl, D, 6 * D], F32, kind="ExternalInput").ap()
    dr["ffn_w_up"] = nc.dram_tensor("ffn_w_up", [nl, D, 2 * DFF], F32, kind="ExternalInput").ap()
    dr["ffn_w_down"] = nc.dram_tensor("ffn_w_down", [nl, DFF, D], F32, kind="ExternalInput").ap()
    if sc_layers:
        dr["sc_w_in"] = nc.dram_tensor("sc_w_in", [len(sc_layers), D, 3 * D], F32, kind="ExternalInput").ap()
        dr["sc_w_out"] = nc.dram_tensor("sc_w_out", [len(sc_layers), D, D], F32, kind="ExternalInput").ap()
    if ml_layers:
        dr["ml_w_in"] = nc.dram_tensor("ml_w_in", [len(ml_layers), D, ML_IN], F32, kind="ExternalInput").ap()
        dr["ml_w_out"] = nc.dram_tensor("ml_w_out", [len(ml_layers), D, D], F32, kind="ExternalInput").ap()
    yT = nc.dram_tensor("yT", [D, T], F32, kind="ExternalOutput").ap()

    with ExitStack() as es:
        S = Sched(nc, es)

        def sb(name, shape, dt):
            return es.enter_context(nc.sbuf_tensor(name, shape, dt))
        x_sb = sb("x_sb", [128, NCH, T], F32)
        h_sb = sb("h_sb", [128, NCH, HALF], BF16)
        act_sb = sb("act_sb", [128, 22 * HALF], BF16)
        ybuf = sb("ybuf", [128, 8192], BF16)
        tmpU = sb("tmpU", [128, 2 * 4 * 1026], BF16)
        ring = sb("ring", [128, NS, NCH, 512], BF16)
        params = sb("params_sb", [128, NP_COLS], F32)
        consts = sb("consts_sb", [128, 512], F32)
        ident_bf = sb("ident_bf", [128, 128], BF16)
        ones_bf = sb("ones_bf", [128, 128], BF16)
        tri_bf = sb("tri_bf", [128, 128], BF16)
        mods = sb("mods", [128, 2, 48], F32)
        condT = sb("condT", [128, NCH], BF16)
        halo_pr = sb("halo_pr", [128, NCH, 2], BF16)
        halo_u = sb("halo_u", [128, NUP, 2], BF16)
        C32 = sb("C32", [128, HEADS, VA], F32)
        Cbf = sb("Cbf", [128, HEADS, VA], BF16)
        ps = es.enter_context(nc.psum_tensor("ps", [128, 8, 512], F32))

        ident_f = consts[:, 0:128]
        tri_f = consts[:, 128:256]
        posmask_f = consts[:, 256:384]
        ones_f = consts[:, 384:512]

        def view(t, boff, shape, dt):
            n = 1
            for s in shape:
                n *= s
            nb = n * DT_SIZE[dt]
            a = t[:, boff // 2:(boff + nb) // 2]
            if dt != BF16:
                a = a.bitcast(dt)
            if len(shape) == 1:
                return a
            names = " ".join("d%d" % i for i in range(len(shape)))
            kw = {"d%d" % i: shape[i] for i in range(len(shape) - 1)}
            return a.rearrange("p (%s) -> p %s" % (names, names), **kw)

        actv = view(act_sb, 0, [22, HALF], BF16)
        U = [view(tmpU, i * 8208, [4, 1026], BF16) for i in range(2)]
        rs_v = view(ybuf, 0, [2, 512], F32)
        tmpn_v = view(ybuf, 4096, [2, 512], F32)
        accv = view(ybuf, 0, [4, 1024], BF16)
        accg = view(ybuf, 8192, [4, 1024], BF16)
        yb_sc = view(ybuf, 0, [NCH, HALF], BF16)
        yb_ml = view(ybuf, 0, [8, 8, 128], BF16)
        sc_gc = view(act_sb, 0, [4, 1024], BF16)
        sc_pr = view(act_sb, 8192, [4, 1026], BF16)
        sc_cv = view(act_sb, 16400, [4, 1024], BF16)
        kv = view(act_sb, 0, [8, KVW], BF16)
        kvv = kv[:, :, 512:].rearrange("p c (h v) -> p c h v", h=HEADS)
        so_v = view(act_sb, 8 * KVW * 2, [2, 512], BF16)
        o_ = [0]

        def carve(shape, dt):
            n = 1
            for s in shape:
                n *= s
            nb = (n * DT_SIZE[dt] + 3) // 4 * 4
            v = view(tmpU, o_[0], shape, dt)
            o_[0] += nb
            return v
        G1 = carve([64], F32)
        TH = carve([64], F32)
        IGt = carve([8, 4], F32)
        Lp = carve([8, 4], F32)
        E4 = carve([8, 4], F32)
        X16 = carve([2, 16], F32)
        EX = carve([8, 16], F32)
        AT = carve([2, HEADS, 128], BF16)
        KW = carve([2, HEADS, 128], BF16)
        HH = carve([3, HEADS, DV], BF16)
        DN = carve([8, 4], F32)
        RR = carve([8, 4], F32)
        SSQ = carve([8, 4], F32)
        SQ = carve([8, 4], F32)
        JUNK = carve([HEADS, DV], BF16)
        NUM = view(act_sb, 8 * KVW * 2 + 2048, [8, HEADS, VA], BF16)
        assert o_[0] <= 2 * 8208, o_[0]

        ring_sem = [S.dma_sem("ring%d" % i) for i in range(NS)]
        ld_sem = [S.dma_sem("ld%d" % i) for i in range(NCH)]
        misc_sem = S.dma_sem("ldmisc")
        misc2_sem = S.dma_sem("ldmisc2")
        st_sem = S.dma_sem("st")

        S.dma("sp", params[:, :], dr["params"], misc_sem)
        S.dma("sp", consts[:, :], dr["consts"], misc2_sem)
        xTv = dr["xT"].rearrange("(c p) t -> p c t", p=128)
        for c in range(NCH):
            S.dma("sp", x_sb[:, c, :], xTv[:, c, :], ld_sem[c])
        S.copy(ident_bf[:, :], ident_f)
        S.copy(ones_bf[:, :], ones_f)
        S.copy(tri_bf[:, :], tri_f)
        S.act(condT[:, :], params[:, P_C:P_C + 8], AF.Silu)

        blocks = []
        state = {"issued": 0, "consumed": 0}

        def issue_next():
            i = state["issued"]
            src, kc, ncols = blocks[i]
            s = i % NS
            S.dma("pool", ring[:, s, 0:kc, 0:ncols], src, ring_sem[s], writes=[ring[:, s, :, :]])
            state["issued"] += 1

        def wblock(w_ap, r0, nrows, c0, ncols):
            src = w_ap[r0:r0 + nrows, c0:c0 + ncols].rearrange("(kc p) n -> p kc n", p=128)
            blocks.append((src, nrows // 128, ncols))
            return len(blocks) - 1

        def use_block(bi):
            assert bi == state["consumed"], (bi, state["consumed"])
            while state["issued"] < min(len(blocks), bi + NS):
                issue_next()
            state["consumed"] += 1
            return ring[:, bi % NS, :, :]

        def done_block(bi):
            while state["issued"] < min(len(blocks), bi + 1 + NS):
                issue_next()

        bank_rr = [0]

        BANKS = (0, 1, 2, 3, 5, 6, 7)

        def next_bank():
            b = BANKS[bank_rr[0]]
            bank_rr[0] = (bank_rr[0] + 1) % len(BANKS)
            return ps[:, b, :]

        def pcol(col):
            return params[:, col:col + 1]

        def wchunks(slot, m, nk=NCH):
            return [slot[:, k, m * 128:(m + 1) * 128] for k in range(nk)]

        def hcols(sl):
            return [h_sb[:, k, sl] for k in range(NCH)]

        def ada_parts(li):
            l = layer_ids[li]
            slot_l = l % 2
            pb = l * PL
            psa = ps[:, 4, 448:496]

            def reg(j):
                return wblock(dr["ada_w"][li], 0, D, j * 512, 512)

            def consume(j, bi):
                slot = use_block(bi)
                for m in range(4):
                    q = 4 * j + m
                    S.group(psa[:, q:q + 1], wchunks(slot, m), [condT[:, k:k + 1] for k in range(NCH)])
                done_block(bi)

            def finalize():
                md = mods[:, slot_l, :]
                S.tt(md, psa, params[:, pb + P_ADAB:pb + P_ADAB + 48], ALU.add)
                for (o_sc, o_g, pg) in ((8, 16, P_NMG), (32, 40, P_NFG)):
                    a_sc = mods[:, slot_l, o_sc:o_sc + 8]
                    a_g = mods[:, slot_l, o_g:o_g + 8]
                    S.stt(a_sc, a_sc, 1.0, params[:, pb + pg:pb + pg + 8], ALU.add, ALU.mult)
                    S.ts(a_g, a_g, 1.0, None, ALU.add)
            return reg, consume, finalize

        def ada_phase(li):
            reg, consume, finalize = ada_parts(li)
            bis = [reg(j) for j in range(12)]

            def run():
                for j, bi in enumerate(bis):
                    consume(j, bi)
                finalize()
            return run

        def rstd_tile(rsv, tok_cols, sq_dst_fn):
            for c in range(NCH):
                S.act(sq_dst_fn(c), x_sb[:, c, tok_cols], AF.Square)
            pn = next_bank()
            S.group(pn, [ones_bf[:, :]] * NCH, [sq_dst_fn(c) for c in range(NCH)])
            S.act(rsv, pn, AF.Sqrt, scale=1.0 / D, bias=eps_col)
            S.recip(rsv, rsv)

        def norm_steps(hf, gm_ap, sh_ap):
            steps = []
            for t2 in range(2):
                tok = slice(hf * HALF + t2 * TT, hf * HALF + (t2 + 1) * TT)
                hs = slice(t2 * TT, (t2 + 1) * TT)
                rsv = rs_v[:, t2, :]

                def s1(tok=tok, hs=hs):
                    for c in range(NCH):
                        S.act(h_sb[:, c, hs], x_sb[:, c, tok], AF.Square)

                def s2(hs=hs, rsv=rsv):
                    pn = next_bank()
                    S.group(pn, [ones_bf[:, :]] * NCH, [h_sb[:, c, hs] for c in range(NCH)])
                    S.act(rsv, pn, AF.Sqrt, scale=1.0 / D, bias=eps_col)
                    S.recip(rsv, rsv)

                def s3(tok=tok, hs=hs, rsv=rsv):
                    for c in range(NCH):
                        tn = tmpn_v[:, c % 2, :]
                        S.stt(tn, x_sb[:, c, tok], gm_ap[:, c:c + 1], rsv, ALU.mult, ALU.mult)
                        S.act(h_sb[:, c, hs], tn, AF.Identity, bias=sh_ap[:, c:c + 1], scale=1.0)
                steps += [s1, s2, s3]
            return steps

        def norm_phase(hf, gm_ap, sh_ap):
            for st in norm_steps(hf, gm_ap, sh_ap):
                st()

        def xupdate(pb_ap, mc, hf, t2, gate_ap):
            tok = slice(hf * HALF + t2 * TT, hf * HALF + (t2 + 1) * TT)
            xs = x_sb[:, mc, tok]
            S.stt(xs, pb_ap, gate_ap[:, mc:mc + 1], xs, ALU.mult, ALU.add)

        def conv3(dst, src, o, w_cols, bias_col):
            w0, w1, w2 = w_cols
            if bias_col is None:
                S.ts(dst, src[:, o + 2:o + 2 + TT], w2, None, ALU.mult)
            else:
                S.ts(dst, src[:, o + 2:o + 2 + TT], w2, bias_col, ALU.mult, ALU.add)
            S.stt(dst, src[:, o + 1:o + 1 + TT], w1, dst, ALU.mult, ALU.add)
            S.stt(dst, src[:, o:o + TT], w0, dst, ALU.mult, ALU.add)

        def halo_in(dst2, halo_ap, hf):
            if hf == 0:
                S.memset(dst2, 0.0)
            else:
                S.copy(dst2, halo_ap)

        def outproj(wo, hf, md, rhs_fn):
            gate = md[:, 16:24]
            for cg, bi in enumerate(wo):
                slot = use_block(bi)
                for m in range(4):
                    mc = 4 * cg + m
                    for t2 in range(2):
                        pb_ap = next_bank()
                        S.group(pb_ap, wchunks(slot, m), [rhs_fn(k, t2) for k in range(NCH)])
                        xupdate(pb_ap, mc, hf, t2, gate)
                done_block(bi)

        def shortconv_phase(li, hf, md):
            l = layer_ids[li]
            j = sc_layers.index(l)
            pb = l * PL
            w_in = dr["sc_w_in"][j]
            seq = []
            for i in range(2):
                seq.append(("gc", i, wblock(w_in, 0, D, D + 512 * i, 512)))
                seq.append(("u", i, wblock(w_in, 0, D, 2 * D + 512 * i, 512)))
                seq.append(("gb", i, wblock(w_in, 0, D, 512 * i, 512)))
            wo = [wblock(dr["sc_w_out"][j], 0, D, 512 * cg, 512) for cg in range(2)]

            def run():
                for kind, i, bi in seq:
                    slot = use_block(bi)
                    for m in range(4):
                        f = 4 * i + m
                        if kind == "u":
                            halo_in(sc_pr[:, m, 0:2], halo_pr[:, f, :], hf)
                        for t2 in range(2):
                            hs = slice(t2 * TT, (t2 + 1) * TT)
                            pb_ap = next_bank()
                            S.group(pb_ap, wchunks(slot, m), hcols(hs))
                            if kind == "gc":
                                S.act(sc_gc[:, m, hs], pb_ap, AF.Copy)
                            elif kind == "u":
                                S.tt(sc_pr[:, m, 2 + t2 * TT:2 + (t2 + 1) * TT], pb_ap, sc_gc[:, m, hs], ALU.mult)
                                wc = [pcol(pb + P_MIX + jj * 8 + f) for jj in range(3)]
                                conv3(sc_cv[:, m, hs], sc_pr[:, m, :], t2 * TT, wc, None)
                                if t2 == 1:
                                    S.copy(halo_pr[:, f, :], sc_pr[:, m, 1024:1026])
                            else:
                                S.tt(yb_sc[:, f, hs], pb_ap, sc_cv[:, m, hs], ALU.mult)
                    done_block(bi)
                outproj(wo, hf, md, lambda k, t2: yb_sc[:, k, t2 * TT:(t2 + 1) * TT])
            return run

        def ffn_phase(li, hf, md, ada_li=None, next_norm=None):
            l = layer_ids[li]
            pb = l * PL
            w_up = dr["ffn_w_up"][li]
            w_dn = dr["ffn_w_down"][li]
            seq = []
            ada = ada_parts(ada_li[0]) if ada_li is not None else None
            for i in range(6):
                ncols = 512 if i < 5 else 256
                seq.append(("v", i, ncols, wblock(w_up, 0, D, DFF + 512 * i, ncols), None, None))
                aj = ada_li[1] + i if ada else None
                seq.append(("g", i, ncols, wblock(w_up, 0, D, 512 * i, ncols), aj, ada[0](aj) if ada else None))
            dseq = []
            for cg in range(2):
                for kb in range(3):
                    nk = 8 if kb < 2 else 6
                    dseq.append((cg, kb, nk, wblock(w_dn, kb * 1024, nk * 128, cg * 512, 512)))

            def run():
                for si, (kind, i, ncols, bi, aj, abi) in enumerate(seq):
                    slot = use_block(bi)
                    Ub = U[0] if kind == "v" else U[1]
                    for m in range(ncols // 128):
                        jj = 4 * i + m
                        q = jj + (22 if kind == "v" else 0)
                        halo_in(Ub[:, m, 0:2], halo_u[:, q, :], hf)
                        w0, w1, w2 = [pcol(pb + P_FCW + t * NUP + q) for t in range(3)]
                        bc = pcol(pb + P_FCB + q)
                        accs = []
                        for t2 in range(2):
                            hs = slice(t2 * TT, (t2 + 1) * TT)
                            pb_ap = next_bank()
                            S.group(pb_ap, wchunks(slot, m), hcols(hs))
                            S.act(Ub[:, m, 2 + t2 * TT:2 + (t2 + 1) * TT], pb_ap, AF.Copy)
                            acc = (accv if kind == "v" else accg)[:, m, hs]
                            if kind == "v":
                                S.act(acc, pb_ap, AF.Identity, scale=w2, bias=bc)
                            accs.append(acc)
                        if kind == "g":
                            for t2 in range(2):
                                o = t2 * TT
                                S.ts(accs[t2], Ub[:, m, o + 2:o + 2 + TT], w2, bc, ALU.mult, ALU.add)
                        for t2 in range(2):
                            o = t2 * TT
                            S.stt(accs[t2], Ub[:, m, o + 1:o + 1 + TT], w1, accs[t2], ALU.mult, ALU.add)
                        for t2 in range(2):
                            o = t2 * TT
                            S.stt(accs[t2], Ub[:, m, o:o + TT], w0, accs[t2], ALU.mult, ALU.add)
                        S.copy_pool(halo_u[:, q, :], Ub[:, m, 1024:1026])
                        if kind == "g":
                            for t2 in range(2):
                                S.act(accs[t2], accs[t2], AF.Silu)
                            for t2 in range(2):
                                hs = slice(t2 * TT, (t2 + 1) * TT)
                                S.tt_pool(actv[:, jj, hs], accs[t2], accv[:, m, hs], ALU.mult)
                    done_block(bi)
                    if abi is not None:
                        ada[1](aj, abi)
                if ada and ada_li[2]:
                    ada[2]()
                gate = md[:, 40:48]
                side = norm_steps(*next_norm) if next_norm is not None else []
                for di, (cg, kb, nk, bi) in enumerate(dseq):
                    slot = use_block(bi)
                    for m in range(4):
                        mc = 4 * cg + m
                        for t2 in range(2):
                            hs = slice(t2 * TT, (t2 + 1) * TT)
                            pb_ap = next_bank()
                            S.group(pb_ap, wchunks(slot, m, nk), [actv[:, kb * 8 + k, hs] for k in range(nk)])
                            xupdate(pb_ap, mc, hf, t2, gate)
                    done_block(bi)
                    if di < len(side):
                        side[di]()
            return run

        def mlstm_phase(li, hf, md):
            l = layer_ids[li]
            j = ml_layers.index(l)
            pb = l * PL
            w_in = dr["ml_w_in"][j]
            b_g = wblock(w_in, 0, D, 3072, 8)
            b_q = wblock(w_in, 0, D, 0, 512)
            b_k = wblock(w_in, 0, D, 512, 512)
            b_v = [wblock(w_in, 0, D, 1024 + 512 * i, 512) for i in range(2)]
            b_o = [wblock(w_in, 0, D, 2048 + 512 * i, 512) for i in range(2)]
            wo = [wblock(dr["ml_w_out"][j], 0, D, 512 * cg, 512) for cg in range(2)]

            def run():
                if hf == 0:
                    S.memset(C32[:, :, :], 0.0)
                    S.memset(Cbf[:, :, :], 0.0)
                S.memset(kvv[:, :, :, DV:VA], 1.0, writes=[kv[:, :, 512:]])
                slot = use_block(b_g)
                pg = ps[:, 4, 384:448]
                for c8 in range(8):
                    S.group(pg[:, c8 * 8:(c8 + 1) * 8], hcols(slice(c8 * 128, (c8 + 1) * 128)),
                            [slot[:, k, 0:8] for k in range(NCH)])
                done_block(b_g)
                S.tt(G1, pg, params[:, pb + P_MIX + 8:pb + P_MIX + 72], ALU.add)
                S.act(TH, G1, AF.Tanh, scale=1.0 / 15.0)
                th3 = TH.rearrange("p (c g) -> p c g", c=8)
                S.ts(IGt, th3[:, :, 0:4], 15.0, None, ALU.mult)
                S.act(E4, th3[:, :, 4:8], AF.Exp, scale=-15.0)
                S.act(Lp, E4, AF.Ln, bias=one_col, scale=1.0)
                for which, bi in (("q", b_q), ("k", b_k)):
                    slot = use_block(bi)
                    for m in range(HEADS):
                        for t2 in range(2):
                            hs = slice(t2 * TT, (t2 + 1) * TT)
                            pb_ap = next_bank()
                            S.group(pb_ap, wchunks(slot, m), hcols(hs))
                            d_ = yb_ml[:, t2 * 4:(t2 + 1) * 4, (m if which == "q" else 4 + m), :]
                            src = pb_ap.rearrange("p (a b) -> p a b", a=4)
                            S.act(d_, src, AF.Copy, scale=(DK ** -0.5 if which == "q" else 1.0))
                    if which == "k":
                        for c8 in range(8):
                            pb_ap = next_bank()
                            S.group(pb_ap, hcols(slice(c8 * 128, (c8 + 1) * 128)), [slot[:, k, 0:512] for k in range(NCH)])
                            S.act(kv[:, c8, 0:512], pb_ap, AF.Copy)
                    done_block(bi)
                for i, bi in enumerate(b_v):
                    slot = use_block(bi)
                    for c8 in range(8):
                        pb_ap = next_bank()
                        S.group(pb_ap, hcols(slice(c8 * 128, (c8 + 1) * 128)), [slot[:, k, 0:512] for k in range(NCH)])
                        S.act(kvv[:, c8, 2 * i:2 * i + 2, 0:DV], pb_ap.rearrange("p (a b) -> p a b", a=2), AF.Copy)
                    done_block(bi)
                pbn = ps[:, 4, 0:8]

                def stage_a(c8):
                    par = c8 % 2
                    x16 = X16[:, par, :]
                    ex = EX[:, c8, :]
                    S.mm(pbn[:, 0:4], tri_f, Lp[:, c8, :], True, True, True)
                    S.mm(pbn[:, 4:8], ones_f, Lp[:, c8, :], True, True, True)
                    S.tt(x16[:, 0:4], IGt[:, c8, :], pbn[:, 0:4], ALU.add)
                    S.copy(x16[:, 8:12], pbn[:, 0:4])
                    S.ts(x16[:, 12:16], pbn[:, 4:8], -1.0, None, ALU.mult)
                    S.tt(x16[:, 4:8], x16[:, 0:4], pbn[:, 4:8], ALU.subtract)
                    S.act(ex, x16, AF.Exp)
                    pS = ps[:, 5 if par == 0 else 7, :].rearrange("p (h t) -> p h t", h=HEADS)
                    for h in range(HEADS):
                        S.mm(pS[:, h, :], yb_ml[:, c8, 4 + h, :], yb_ml[:, c8, h, :], True, True, True)
                    for h in range(HEADS):
                        S.stt(AT[:, par, h, :], pS[:, h, :], ex[:, h:h + 1], tri_bf[:, :], ALU.mult, ALU.mult)
                    for h in range(HEADS):
                        S.ts(KW[:, par, h, :], kv[:, c8, h * 128:(h + 1) * 128], ex[:, 4 + h:5 + h], None, ALU.mult)
                    for h in range(HEADS):
                        pn_ = ps[:, h, 0:VA]
                        S.mm(pn_, AT[:, par, h, :], kvv[:, c8, h, :], True, False, False)
                        S.mm(pn_, yb_ml[:, c8, h, :], Cbf[:, h, :], False, True, True)
                    for h in range(HEADS):
                        S.act(NUM[:, c8, h, :], ps[:, h, 0:VA], AF.Copy)
                    for h in range(HEADS):
                        S.mm(ps[:, h, 0:VA], KW[:, par, h, :], kvv[:, c8, h, :], True, True, True)
                    for h in range(HEADS):
                        S.stt(C32[:, h, :], C32[:, h, :], ex[:, 12 + h:13 + h], ps[:, h, 0:VA], ALU.mult, ALU.add)
                    for h in range(HEADS):
                        S.act(Cbf[:, h, :], C32[:, h, :], AF.Copy)
                    for h in range(HEADS):
                        nv = NUM[:, c8, h, 0:DV]
                        if h < 2:
                            S.act(JUNK[:, h, :], nv, AF.Square, accum_out=SSQ[:, c8, h:h + 1])
                        else:
                            S.op("dve", lambda e, nv=nv, jk=JUNK[:, h, :], acc=SSQ[:, c8, h:h + 1]: e.scalar_tensor_tensor(
                                out=jk, in0=nv, scalar=1.0, in1=nv, op0=ALU.mult, op1=ALU.mult, accum_out=acc),
                                [nv], [JUNK[:, h, :], SSQ[:, c8, h:h + 1]])

                for c8 in range(8):
                    stage_a(c8)

                S.act(DN, NUM[:, :, :, DV], AF.Abs)
                S.tt(DN, DN, EX[:, :, 8:12], ALU.max)
                S.recip(DN, DN)
                S.tt(SSQ, SSQ, DN, ALU.mult)
                S.tt(SSQ, SSQ, DN, ALU.mult)
                S.act(SQ, SSQ, AF.Ln, scale=1.0 / DV, bias=eps_col)
                S.act(SQ, SQ, AF.Exp, scale=-0.5)
                S.tt(RR, SQ, DN, ALU.mult)
                for c8 in range(8):
                    hb = c8 % 3
                    for h in range(HEADS):
                        S.ts(HH[:, hb, h, :], NUM[:, c8, h, 0:DV], RR[:, c8, h:h + 1], None, ALU.mult)
                    pTb = ps[:, (5, 6, 7)[c8 % 3], :].bitcast(BF16).rearrange("p (h j t) -> p h j t", h=HEADS, j=2)
                    for h in range(HEADS):
                        for jj in range(2):
                            S.transpose(pTb[:, h, jj, :], HH[:, hb, h, jj * 128:(jj + 1) * 128], ident_bf[:, :])
                    on_act = (c8 % 3 == 2)
                    for h in range(HEADS):
                        for jj in range(2):
                            gcol = pcol(pb + P_MIX + 2 * h + jj)
                            if on_act:
                                S.act(yb_ml[:, c8, 2 * h + jj, :], pTb[:, h, jj, :], AF.Copy, scale=gcol)
                            else:
                                S.ts(yb_ml[:, c8, 2 * h + jj, :], pTb[:, h, jj, :], gcol, None, ALU.mult)
                for i, bi in enumerate(b_o):
                    slot = use_block(bi)
                    for m in range(4):
                        f = 4 * i + m
                        for t2 in range(2):
                            hs = slice(t2 * TT, (t2 + 1) * TT)
                            pb_ap = next_bank()
                            S.group(pb_ap, wchunks(slot, m), hcols(hs))
                            so = so_v[:, t2, :]
                            S.act(so, pb_ap, AF.Sigmoid)
                            d_ = yb_ml[:, t2 * 4:(t2 + 1) * 4, f, :]
                            S.tt(d_, d_, so.rearrange("p (a b) -> p a b", a=4), ALU.mult)
                    done_block(bi)
                outproj(wo, hf, md, lambda k, t2: yb_ml[:, t2 * 4:(t2 + 1) * 4, k, :])
            return run

        eps_col = sb("eps_col", [128, 1], F32)
        one_col = sb("one_col", [128, 1], F32)
        S.memset(eps_col[:, :], EPS)
        S.memset(one_col[:, :], 1.0)
        eps_col = eps_col[:, :]
        one_col = one_col[:, :]

        sequence = [ada_phase(0)]
        for li, l in enumerate(layer_ids):
            md = mods[:, l % 2, :]
            for hf in range(2):
                if li == 0 and hf == 0:
                    sequence.append(("norm", hf, md[:, 8:16], md[:, 0:8]))
                if l % 2 == 0:
                    sequence.append(shortconv_phase(li, hf, md))
                else:
                    sequence.append(mlstm_phase(li, hf, md))
                sequence.append(("norm", hf, md[:, 32:40], md[:, 24:32]))
                if hf == 0:
                    nn = (1, md[:, 8:16], md[:, 0:8])
                elif li + 1 < nl:
                    md2 = mods[:, layer_ids[li + 1] % 2, :]
                    nn = (0, md2[:, 8:16], md2[:, 0:8])
                else:
                    nn = None
                sequence.append(ffn_phase(li, hf, md, ada_li=((li + 1, 6 * hf, hf == 1) if li + 1 < nl else None), next_norm=nn))
        for item in sequence:
            if isinstance(item, tuple):
                norm_phase(item[1], item[2], item[3])
            else:
                item()

        yTv = yT.rearrange("(c p) t -> p c t", p=128)
        if final_norm:
            fg = params[:, P_FNG:P_FNG + 8]
            for tt in range(4):
                tok = slice(tt * TT, (tt + 1) * TT)
                rsv = rs_v[:, tt % 2, :]
                rstd_tile(rsv, tok, lambda c: h_sb[:, c, 0:TT])
                for c in range(NCH):
                    xs = x_sb[:, c, tok]
                    S.stt(xs, xs, fg[:, c:c + 1], rsv, ALU.mult, ALU.mult)
        tok_last = None
        for c in range(NCH):
            tok_last = S.dma("sp", yTv[:, c, :], x_sb[:, c, :], st_sem)
        S.wait_only("sp", [tok_last])
        S.emit()
        nc._sched_stats = S.stats
    return nc


def _consts():
    c = np.zeros((128, 512), np.float32)
    c[:, 0:128] = np.eye(128, dtype=np.float32)
    j = np.arange(128)[:, None]
    t = np.arange(128)[None, :]
    c[:, 128:256] = (j <= t).astype(np.float32)
    c[:, 256:384] = np.where(j <= t, 0.0, 30000.0)
    c[:, 384:512] = 1.0
    return c


def _chunked(v):
    v = np.asarray(v, np.float32)
    return np.ascontiguousarray(v.reshape(-1, 128).T)


def _params_for_core(b, inp):
    P = np.zeros((128, NP_COLS), np.float32)
    for l in range(DEPTH):
        pb = l * PL
        j = l // 2
        P[:, pb + P_ADAB:pb + P_ADAB + 48] = _chunked(inp["ada_b"][l])
        P[:, pb + P_NMG:pb + P_NMG + 8] = _chunked(inp["norm_mix_g"][l])
        P[:, pb + P_NFG:pb + P_NFG + 8] = _chunked(inp["norm_ffn_g"][l])
        if l % 2 == 0:
            for t in range(3):
                P[:, pb + P_MIX + t * 8:pb + P_MIX + (t + 1) * 8] = _chunked(inp["sc_conv_w"][j][t])
        else:
            P[:, pb + P_MIX:pb + P_MIX + 8] = _chunked(inp["ml_norm_g"][j])
            bif = np.concatenate([np.asarray(inp["ml_b_i"][j], np.float32), np.asarray(inp["ml_b_f"][j], np.float32)])
            P[:, pb + P_MIX + 8:pb + P_MIX + 72] = np.tile(bif, 8)[None, :]
        for t in range(3):
            P[:, pb + P_FCW + t * NUP:pb + P_FCW + (t + 1) * NUP] = _chunked(inp["ffn_conv_w"][l][t])
        P[:, pb + P_FCB:pb + P_FCB + NUP] = _chunked(inp["ffn_conv_b"][l])
    P[:, P_C:P_C + 8] = _chunked(inp["c"][b])
    P[:, P_FNG:P_FNG + 8] = _chunked(inp["final_norm_g"])
    return P


_PROG_CACHE = {}


def _get_prog(layer_ids, final_norm):
    key = (tuple(layer_ids), final_norm)
    if key not in _PROG_CACHE:
        _PROG_CACHE[key] = build_program(list(layer_ids), final_norm)
    return _PROG_CACHE[key]


def _launch(layer_ids, final_norm, xT_list, inp, params_list, consts):
    nc = _get_prog(layer_ids, final_norm)
    l0, l1 = layer_ids[0], layer_ids[-1] + 1
    sc = [l // 2 for l in layer_ids if l % 2 == 0]
    ml = [l // 2 for l in layer_ids if l % 2 == 1]
    shared = {
        "consts": consts,
        "ada_w": np.ascontiguousarray(inp["ada_w"][l0:l1]),
        "ffn_w_up": np.ascontiguousarray(inp["ffn_w_up"][l0:l1]),
        "ffn_w_down": np.ascontiguousarray(inp["ffn_w_down"][l0:l1]),
    }
    if sc:
        shared["sc_w_in"] = np.ascontiguousarray(inp["sc_w_in"][sc[0]:sc[-1] + 1])
        shared["sc_w_out"] = np.ascontiguousarray(inp["sc_w_out"][sc[0]:sc[-1] + 1])
    if ml:
        shared["ml_w_in"] = np.ascontiguousarray(inp["ml_w_in"][ml[0]:ml[-1] + 1])
        shared["ml_w_out"] = np.ascontiguousarray(inp["ml_w_out"][ml[0]:ml[-1] + 1])
    in_maps = []
    for b in range(8):
        m = dict(shared)
        m["xT"] = xT_list[b]
        m["params"] = params_list[b]
        in_maps.append(m)
    res = run_bass_kernel_spmd(nc, in_maps, core_ids=list(range(8)))
    return [np.asarray(r["yT"]) for r in res.results]


LAUNCH_GROUPS = [[0, 1, 2, 3]]


def kernel(**inputs):
    inp = {k: np.asarray(v) for k, v in inputs.items()}
    x = inp["x"].astype(np.float32, copy=False)
    consts = _consts()
    params_list = [_params_for_core(b, inp) for b in range(8)]
    xT_list = [np.ascontiguousarray(x[b].T) for b in range(8)]
    for gi, grp in enumerate(LAUNCH_GROUPS):
        xT_list = _launch(grp, gi == len(LAUNCH_GROUPS) - 1, xT_list, inp, params_list, consts)
    out = np.stack([np.ascontiguousarray(xT_list[b].T) for b in range(8)], axis=0)
    return out.astype(np.float32, copy=False)
```

```python
import numpy as np
from contextlib import ExitStack
import concourse.bass as bass
import concourse.mybir as mybir
from concourse.bass_utils import run_bass_kernel_spmd

F32 = mybir.dt.float32
BF16 = mybir.dt.bfloat16
AF = mybir.ActivationFunctionType
ALU = mybir.AluOpType
DT_SIZE = {F32: 4, BF16: 2}

D = 1024
T = 2048
DEPTH = 4
DFF = 2816
NUP = 2 * DFF // 128
NCH = 8
HALF = 1024
TT = 512
NS = 4
EPS = 1e-6
HEADS = 4
DV = 256
DK = 128
VA = DV + 1
KVW = 512 + HEADS * VA
ML_IN = 3080

PL = 320
P_ADAB, P_NMG, P_NFG, P_MIX, P_FCW, P_FCB = 0, 48, 56, 64, 136, 268
P_C = DEPTH * PL
P_FNG = P_C + 8
NP_COLS = P_FNG + 8


def _isap(v):
    return hasattr(v, "tensor") and hasattr(v, "ap")


def _is_dram(ap):
    return "DRAM" in str(ap.space).upper()


def _is_psum(ap):
    return "PSUM" in str(ap.space).upper()


def ap_ranges(ap):
    dsz = DT_SIZE[ap.dtype]
    dims = [list(d) for d in ap.ap]
    pstride = dims[0][0]
    off = ap.offset % pstride if pstride else ap.offset
    free = dims[1:]
    if not free:
        return [(off * dsz, (off + 1) * dsz)]
    ls, lc = free[-1]
    span = (lc - 1) * abs(ls) + 1
    outer = free[:-1]
    n_outer = 1
    for s, c in outer:
        n_outer *= c
    offs = [0]
    if n_outer > 256:
        hi = sum((c - 1) * abs(s) for s, c in free) + 1
        return [(off * dsz, (off + hi) * dsz)]
    for s, c in outer:
        offs = [o + i * s for o in offs for i in range(c)]
    ivs = sorted((off + o, off + o + span) for o in offs)
    out = []
    for a, b in ivs:
        if out and a <= out[-1][1]:
            out[-1][1] = max(out[-1][1], b)
        else:
            out.append([a, b])
    return [(a * dsz, b * dsz) for a, b in out]


class Sched:
    ENGS = ("pe", "dve", "act", "pool", "sp")

    def __init__(self, nc, es):
        self.nc = nc
        self.es = es
        self.semobj = {e: es.enter_context(nc.semaphore("s_" + e)) for e in self.ENGS}
        self.cnt = {e: 0 for e in self.ENGS}
        self.ops = {e: [] for e in self.ENGS}
        self.seen = {e: {} for e in self.ENGS}
        self.mem = {}
        self.dmacnt = {}
        self.nops = 0

    def dma_sem(self, name):
        self.semobj[name] = self.es.enter_context(self.nc.semaphore(name))
        self.dmacnt[name] = 0
        return name

    def _access(self, name, lo, hi, write, tok, deps):
        ivs = self.mem.get(name)
        if ivs is None:
            ivs = [[0, 1 << 40, None, {}]]
        new = []
        for iv in ivs:
            a, b, w, r = iv
            if b <= lo or a >= hi:
                new.append(iv)
                continue
            if a < lo:
                new.append([a, lo, w, dict(r)])
                a = lo
            tail = None
            if b > hi:
                tail = [hi, b, w, dict(r)]
                b = hi
            if w is not None and w != tok:
                if deps.get(w[0], 0) < w[1]:
                    deps[w[0]] = w[1]
            if write:
                for k, v in r.items():
                    if (k, v) != tok and deps.get(k, 0) < v:
                        deps[k] = v
                new.append([a, b, tok, {}])
            else:
                r2 = dict(r)
                if r2.get(tok[0], 0) < tok[1]:
                    r2[tok[0]] = tok[1]
                new.append([a, b, w, r2])
            if tail:
                new.append(tail)
        merged = []
        for iv in new:
            if merged and merged[-1][1] == iv[0] and merged[-1][2] == iv[2] and merged[-1][3] == iv[3]:
                merged[-1][1] = iv[1]
            else:
                merged.append(iv)
        self.mem[name] = merged

    def op(self, eng, fn, reads=(), writes=(), mark=True, dma=None, extra_deps=()):
        if dma is not None:
            self.dmacnt[dma] += 16
            tok = (dma, self.dmacnt[dma])
        elif mark:
            self.cnt[eng] += 1
            tok = (eng, self.cnt[eng])
        else:
            tok = (eng, self.cnt[eng] + 1)
        deps = {}
        for k, v in extra_deps:
            if deps.get(k, 0) < v:
                deps[k] = v
        pdeps = {}
        for ap in list(reads) + list(writes):
            if _is_psum(ap):
                for lo, hi in ap_ranges(ap):
                    self._access(ap.tensor.name, lo // 2048 * 2048, (hi + 2047) // 2048 * 2048, True, tok, pdeps)
        for k, v in pdeps.items():
            if k != eng and deps.get(k, 0) < v:
                deps[k] = v
        for ap in reads:
            if not _is_dram(ap) and not _is_psum(ap):
                for lo, hi in ap_ranges(ap):
                    self._access(ap.tensor.name, lo, hi, False, tok, deps)
        for ap in writes:
            if not _is_dram(ap) and not _is_psum(ap):
                for lo, hi in ap_ranges(ap):
                    self._access(ap.tensor.name, lo, hi, True, tok, deps)
        waits = []
        for k, v in deps.items():
            if (k, v) == tok:
                continue
            if k == eng and eng == "pe":
                continue
            if k == eng and v > self.cnt[eng] - (1 if (mark and dma is None) else 0):
                continue
            if self.seen[eng].get(k, 0) >= v:
                continue
            self.seen[eng][k] = v
            waits.append((k, v))
        self.ops[eng].append((waits, fn, tok if (mark or dma is not None) else None))
        self.nops += 1
        return tok

    def wait_only(self, eng, toks):
        waits = []
        for k, v in toks:
            if self.seen[eng].get(k, 0) < v:
                self.seen[eng][k] = v
                waits.append((k, v))
        self.ops[eng].append((waits, None, None))

    def act(self, out, in_, func, **kw):
        reads = [in_] + [v for k, v in kw.items() if _isap(v) and k != "accum_out"]
        writes = [out] + ([kw["accum_out"]] if "accum_out" in kw else [])
        return self.op("act", lambda e: e.activation(out=out, in_=in_, func=func, **kw), reads, writes)

    def tt(self, out, in0, in1, op):
        return self.op("dve", lambda e: e.tensor_tensor(out=out, in0=in0, in1=in1, op=op), [in0, in1], [out])

    def tt_pool(self, out, in0, in1, op):
        return self.op("pool", lambda e: e.tensor_tensor(out=out, in0=in0, in1=in1, op=op), [in0, in1], [out])

    def copy_pool(self, out, in_):
        return self.op("pool", lambda e: e.tensor_copy(out=out, in_=in_), [in_], [out])

    def stt(self, out, in0, scalar, in1, op0, op1):
        reads = [in0, in1] + ([scalar] if _isap(scalar) else [])
        return self.op("dve", lambda e: e.scalar_tensor_tensor(out=out, in0=in0, scalar=scalar, in1=in1, op0=op0, op1=op1),
                       reads, [out])

    def ts(self, out, in0, s1, s2, op0, op1=None):
        reads = [in0] + [s for s in (s1, s2) if _isap(s)]
        if op1 is None:
            return self.op("dve", lambda e: e.tensor_scalar(out=out, in0=in0, scalar1=s1, scalar2=None, op0=op0), reads, [out])
        return self.op("dve", lambda e: e.tensor_scalar(out=out, in0=in0, scalar1=s1, scalar2=s2, op0=op0, op1=op1), reads, [out])

    def copy(self, out, in_):
        return self.op("dve", lambda e: e.tensor_copy(out=out, in_=in_), [in_], [out])

    def memset(self, ap, val, writes=None):
        return self.op("dve", lambda e: e.memset(ap, val), [], [ap] if writes is None else writes)

    def recip(self, out, in_):
        return self.op("dve", lambda e: e.reciprocal(out=out, in_=in_), [in_], [out])

    def mm(self, out, lhsT, rhs, start, stop, mark):
        return self.op("pe", lambda e: e.matmul(out, lhsT=lhsT, rhs=rhs, start=start, stop=stop),
                       [lhsT, rhs], [out], mark=mark)

    def group(self, out, lhs_list, rhs_list):
        n = len(lhs_list)
        for k in range(n):
            self.mm(out, lhs_list[k], rhs_list[k], k == 0, k == n - 1, k == n - 1)

    def transpose(self, out, in_, ident):
        return self.op("pe", lambda e: e.transpose(out, in_, ident), [in_, ident], [out])

    def dma(self, eng, out, in_, sem, writes=None):
        w = writes if writes is not None else ([] if _is_dram(out) else [out])
        r = [] if _is_dram(in_) else [in_]
        return self.op(eng, lambda e: e.dma_start(out=out, in_=in_), r, w, dma=sem)

    def emit(self):
        nc = self.nc
        me = self

        def mk(engname):
            def body(e):
                for waits, fn, tok in me.ops[engname]:
                    for k, v in waits:
                        e.wait_ge(me.semobj[k], v)
                    if fn is None:
                        continue
                    ins = fn(e)
                    if tok is not None:
                        ins.then_inc(me.semobj[tok[0]], 16 if tok[0] in me.dmacnt else 1)
            return body

        self.stats = {e: len(v) for e, v in self.ops.items()}
        with nc.Block() as block:
            block.tensor(mk("pe"))
            block.vector(mk("dve"))
            block.scalar(mk("act"))
            block.gpsimd(mk("pool"))
            block.sync(mk("sp"))


def build_program(layer_ids, final_norm=True):
    nl = len(layer_ids)
    sc_layers = [l for l in layer_ids if l % 2 == 0]
    ml_layers = [l for l in layer_ids if l % 2 == 1]
    nc = bass.Bass("TRN2", target_bir_lowering=False)
    dr = {}
    dr["xT"] = nc.dram_tensor("xT", [D, T], F32, kind="ExternalInput").ap()
    dr["params"] = nc.dram_tensor("params", [128, NP_COLS], F32, kind="ExternalInput").ap()
    dr["consts"] = nc.dram_tensor("consts", [128, 512], F32, kind="ExternalInput").ap()
    dr["ada_w"] = nc.dram_tensor("ada_w", [nl, D, 6 * D], F32, kind="ExternalInput").ap()
    dr["ffn_w_up"] = nc.dram_tensor("ffn_w_up", [nl, D, 2 * DFF], F32, kind="ExternalInput").ap()
    dr["ffn_w_down"] = nc.dram_tensor("ffn_w_down", [nl, DFF, D], F32, kind="ExternalInput").ap()
    if sc_layers:
        dr["sc_w_in"] = nc.dram_tensor("sc_w_in", [len(sc_layers), D, 3 * D], F32, kind="ExternalInput").ap()
        dr["sc_w_out"] = nc.dram_tensor("sc_w_out", [len(sc_layers), D, D], F32, kind="ExternalInput").ap()
    if ml_layers:
        dr["ml_w_in"] = nc.dram_tensor("ml_w_in", [len(ml_layers), D, ML_IN], F32, kind="ExternalInput").ap()
        dr["ml_w_out"] = nc.dram_tensor("ml_w_out", [len(ml_layers), D, D], F32, kind="ExternalInput").ap()
    yT = nc.dram_tensor("yT", [D, T], F32, kind="ExternalOutput").ap()

    with ExitStack() as es:
        S = Sched(nc, es)

        def sb(name, shape, dt):
            return es.enter_context(nc.sbuf_tensor(name, shape, dt))
        x_sb = sb("x_sb", [128, NCH, T], F32)
        h_sb = sb("h_sb", [128, NCH, HALF], BF16)
        act_sb = sb("act_sb", [128, 22 * HALF], BF16)
        ybuf = sb("ybuf", [128, 8192], BF16)
        tmpU = sb("tmpU", [128, 2 * 4 * 1026], BF16)
        ring = sb("ring", [128, NS, NCH, 512], BF16)
        params = sb("params_sb", [128, NP_COLS], F32)
        consts = sb("consts_sb", [128, 512], F32)
        ident_bf = sb("ident_bf", [128, 128], BF16)
        ones_bf = sb("ones_bf", [128, 128], BF16)
        tri_bf = sb("tri_bf", [128, 128], BF16)
        mods = sb("mods", [128, 2, 48], F32)
        condT = sb("condT", [128, NCH], BF16)
        halo_pr = sb("halo_pr", [128, NCH, 2], BF16)
        halo_u = sb("halo_u", [128, NUP, 2], BF16)
        C32 = sb("C32", [128, HEADS, VA], F32)
        Cbf = sb("Cbf", [128, HEADS, VA], BF16)
        ps = es.enter_context(nc.psum_tensor("ps", [128, 8, 512], F32))

        ident_f = consts[:, 0:128]
        tri_f = consts[:, 128:256]
        posmask_f = consts[:, 256:384]
        ones_f = consts[:, 384:512]

        def view(t, boff, shape, dt):
            n = 1
            for s in shape:
                n *= s
            nb = n * DT_SIZE[dt]
            a = t[:, boff // 2:(boff + nb) // 2]
            if dt != BF16:
                a = a.bitcast(dt)
            if len(shape) == 1:
                return a
            names = " ".join("d%d" % i for i in range(len(shape)))
            kw = {"d%d" % i: shape[i] for i in range(len(shape) - 1)}
            return a.rearrange("p (%s) -> p %s" % (names, names), **kw)

        actv = view(act_sb, 0, [22, HALF], BF16)
        U = [view(tmpU, i * 8208, [4, 1026], BF16) for i in range(2)]
        rs_v = view(tmpU, 0, [2, 512], F32)
        tmpn_v = view(tmpU, 4096, [2, 512], F32)
        accv = view(ybuf, 0, [4, 1024], BF16)
        accg = view(ybuf, 8192, [4, 1024], BF16)
        yb_sc = view(ybuf, 0, [NCH, HALF], BF16)
        yb_ml = view(ybuf, 0, [8, 8, 128], BF16)
        sc_gc = view(act_sb, 0, [4, 1024], BF16)
        sc_pr = view(act_sb, 8192, [4, 1026], BF16)
        sc_cv = view(act_sb, 16400, [4, 1024], BF16)
        kv = view(act_sb, 0, [8, KVW], BF16)
        kvv = kv[:, :, 512:].rearrange("p c (h v) -> p c h v", h=HEADS)
        so_v = view(act_sb, 8 * KVW * 2, [2, 512], BF16)
        o_ = [0]

        def carve(shape, dt):
            n = 1
            for s in shape:
                n *= s
            nb = (n * DT_SIZE[dt] + 3) // 4 * 4
            v = view(tmpU, o_[0], shape, dt)
            o_[0] += nb
            return v
        G1 = carve([64], F32)
        TH = carve([64], F32)
        IGt = carve([8, 4], F32)
        Lp = carve([8, 4], F32)
        E4 = carve([8, 4], F32)
        X16 = carve([2, 16], F32)
        EX = carve([8, 16], F32)
        AT = carve([2, HEADS, 128], BF16)
        KW = carve([2, HEADS, 128], BF16)
        HH = carve([3, HEADS, DV], BF16)
        DN = carve([8, 4], F32)
        RR = carve([8, 4], F32)
        SSQ = carve([8, 4], F32)
        SQ = carve([8, 4], F32)
        JUNK = carve([HEADS, DV], BF16)
        NUM = view(act_sb, 8 * KVW * 2 + 2048, [8, HEADS, VA], BF16)
        assert o_[0] <= 2 * 8208, o_[0]

        ring_sem = [S.dma_sem("ring%d" % i) for i in range(NS)]
        ld_sem = [S.dma_sem("ld%d" % i) for i in range(NCH)]
        misc_sem = S.dma_sem("ldmisc")
        misc2_sem = S.dma_sem("ldmisc2")
        st_sem = S.dma_sem("st")

        S.dma("sp", params[:, :], dr["params"], misc_sem)
        S.dma("sp", consts[:, :], dr["consts"], misc2_sem)
        xTv = dr["xT"].rearrange("(c p) t -> p c t", p=128)
        for c in range(NCH):
            S.dma("sp", x_sb[:, c, :], xTv[:, c, :], ld_sem[c])
        S.copy(ident_bf[:, :], ident_f)
        S.copy(ones_bf[:, :], ones_f)
        S.copy(tri_bf[:, :], tri_f)
        S.act(condT[:, :], params[:, P_C:P_C + 8], AF.Silu)

        blocks = []
        state = {"issued": 0, "consumed": 0}

        def issue_next():
            i = state["issued"]
            src, kc, ncols = blocks[i]
            s = i % NS
            S.dma("pool", ring[:, s, 0:kc, 0:ncols], src, ring_sem[s], writes=[ring[:, s, :, :]])
            state["issued"] += 1

        def wblock(w_ap, r0, nrows, c0, ncols):
            src = w_ap[r0:r0 + nrows, c0:c0 + ncols].rearrange("(kc p) n -> p kc n", p=128)
            blocks.append((src, nrows // 128, ncols))
            return len(blocks) - 1

        def use_block(bi, hold=0):
            assert bi == state["consumed"], (bi, state["consumed"])
            while state["issued"] < min(len(blocks), bi + NS - hold):
                issue_next()
            state["consumed"] += 1
            return ring[:, bi % NS, :, :]

        def done_block(bi):
            while state["issued"] < min(len(blocks), bi + 1 + NS):
                issue_next()

        bank_rr = [0]

        BANKS = (0, 1, 2, 3, 5, 6, 7)

        def next_bank():
            b = BANKS[bank_rr[0]]
            bank_rr[0] = (bank_rr[0] + 1) % len(BANKS)
            return ps[:, b, :]

        def pcol(col):
            return params[:, col:col + 1]

        def wchunks(slot, m, nk=NCH):
            return [slot[:, k, m * 128:(m + 1) * 128] for k in range(nk)]

        def hcols(sl):
            return [h_sb[:, k, sl] for k in range(NCH)]

        def ada_parts(li):
            l = layer_ids[li]
            slot_l = l % 2
            pb = l * PL
            psa = ps[:, 4, 448:496]

            def reg(j):
                return wblock(dr["ada_w"][li], 0, D, j * 512, 512)

            def consume(j, bi):
                slot = use_block(bi)
                for m in range(4):
                    q = 4 * j + m
                    S.group(psa[:, q:q + 1], wchunks(slot, m), [condT[:, k:k + 1] for k in range(NCH)])
                done_block(bi)

            def finalize():
                md = mods[:, slot_l, :]
                S.tt(md, psa, params[:, pb + P_ADAB:pb + P_ADAB + 48], ALU.add)
                for (o_sc, o_g, pg) in ((8, 16, P_NMG), (32, 40, P_NFG)):
                    a_sc = mods[:, slot_l, o_sc:o_sc + 8]
                    a_g = mods[:, slot_l, o_g:o_g + 8]
                    S.stt(a_sc, a_sc, 1.0, params[:, pb + pg:pb + pg + 8], ALU.add, ALU.mult)
                    S.ts(a_g, a_g, 1.0, None, ALU.add)
            return reg, consume, finalize

        def ada_phase(li):
            reg, consume, finalize = ada_parts(li)
            bis = [reg(j) for j in range(12)]

            def run():
                for j, bi in enumerate(bis):
                    consume(j, bi)
                finalize()
            return run

        def rstd_tile(rsv, tok_cols, sq_dst_fn):
            for c in range(NCH):
                S.act(sq_dst_fn(c), x_sb[:, c, tok_cols], AF.Square)
            pn = next_bank()
            S.group(pn, [ones_bf[:, :]] * NCH, [sq_dst_fn(c) for c in range(NCH)])
            S.act(rsv, pn, AF.Sqrt, scale=1.0 / D, bias=eps_col)
            S.recip(rsv, rsv)

        def norm_steps(hf, gm_ap, sh_ap):
            steps = []
            for t2 in range(2):
                tok = slice(hf * HALF + t2 * TT, hf * HALF + (t2 + 1) * TT)
                hs = slice(t2 * TT, (t2 + 1) * TT)
                rsv = rs_v[:, t2, :]

                def s1(tok=tok, hs=hs):
                    for c in range(NCH):
                        S.act(h_sb[:, c, hs], x_sb[:, c, tok], AF.Square)

                def s2(hs=hs, rsv=rsv):
                    pn = next_bank()
                    S.group(pn, [ones_bf[:, :]] * NCH, [h_sb[:, c, hs] for c in range(NCH)])
                    S.act(rsv, pn, AF.Sqrt, scale=1.0 / D, bias=eps_col)
                    S.recip(rsv, rsv)

                def s3(tok=tok, hs=hs, rsv=rsv):
                    for c in range(NCH):
                        tn = tmpn_v[:, c % 2, :]
                        S.stt(tn, x_sb[:, c, tok], gm_ap[:, c:c + 1], rsv, ALU.mult, ALU.mult)
                        S.act(h_sb[:, c, hs], tn, AF.Identity, bias=sh_ap[:, c:c + 1], scale=1.0)
                steps += [s1, s2, s3]
            return steps

        def norm_phase(hf, gm_ap, sh_ap):
            for st in norm_steps(hf, gm_ap, sh_ap):
                st()

        def xupdate(pb_ap, mc, hf, t2, gate_ap):
            tok = slice(hf * HALF + t2 * TT, hf * HALF + (t2 + 1) * TT)
            xs = x_sb[:, mc, tok]
            S.stt(xs, pb_ap, gate_ap[:, mc:mc + 1], xs, ALU.mult, ALU.add)

        def conv3(dst, src, o, w_cols, bias_col):
            w0, w1, w2 = w_cols
            if bias_col is None:
                S.ts(dst, src[:, o + 2:o + 2 + TT], w2, None, ALU.mult)
            else:
                S.ts(dst, src[:, o + 2:o + 2 + TT], w2, bias_col, ALU.mult, ALU.add)
            S.stt(dst, src[:, o + 1:o + 1 + TT], w1, dst, ALU.mult, ALU.add)
            S.stt(dst, src[:, o:o + TT], w0, dst, ALU.mult, ALU.add)

        def halo_in(dst2, halo_ap, hf):
            if hf == 0:
                S.memset(dst2, 0.0)
            else:
                S.copy(dst2, halo_ap)

        def outproj(wo, hf, md, rhs_fn):
            gate = md[:, 16:24]
            n2 = norm_steps(hf, md[:, 32:40], md[:, 24:32])
            slots = [use_block(bi, hold=i) for i, bi in enumerate(wo)]

            def groups(t2, cg):
                for m in range(4):
                    mc = 4 * cg + m
                    pb_ap = next_bank()
                    S.group(pb_ap, wchunks(slots[cg], m), [rhs_fn(k, t2) for k in range(NCH)])
                    xupdate(pb_ap, mc, hf, t2, gate)
            groups(0, 0)
            groups(0, 1)
            n2[0]()
            groups(1, 0)
            n2[1]()
            groups(1, 1)
            for bi in wo:
                done_block(bi)
            n2[2]()
            n2[3]()
            n2[4]()
            n2[5]()

        def shortconv_phase(li, hf, md):
            l = layer_ids[li]
            j = sc_layers.index(l)
            pb = l * PL
            w_in = dr["sc_w_in"][j]
            seq = []
            for i in range(2):
                seq.append(("gc", i, wblock(w_in, 0, D, D + 512 * i, 512)))
                seq.append(("u", i, wblock(w_in, 0, D, 2 * D + 512 * i, 512)))
                seq.append(("gb", i, wblock(w_in, 0, D, 512 * i, 512)))
            wo = [wblock(dr["sc_w_out"][j], 0, D, 512 * cg, 512) for cg in range(2)]

            def run():
                for kind, i, bi in seq:
                    slot = use_block(bi)
                    for m in range(4):
                        f = 4 * i + m
                        if kind == "u":
                            halo_in(sc_pr[:, m, 0:2], halo_pr[:, f, :], hf)
                        for t2 in range(2):
                            hs = slice(t2 * TT, (t2 + 1) * TT)
                            pb_ap = next_bank()
                            S.group(pb_ap, wchunks(slot, m), hcols(hs))
                            if kind == "gc":
                                S.act(sc_gc[:, m, hs], pb_ap, AF.Copy)
                            elif kind == "u":
                                S.tt(sc_pr[:, m, 2 + t2 * TT:2 + (t2 + 1) * TT], pb_ap, sc_gc[:, m, hs], ALU.mult)
                                wc = [pcol(pb + P_MIX + jj * 8 + f) for jj in range(3)]
                                conv3(sc_cv[:, m, hs], sc_pr[:, m, :], t2 * TT, wc, None)
                                if t2 == 1:
                                    S.copy(halo_pr[:, f, :], sc_pr[:, m, 1024:1026])
                            else:
                                S.tt(yb_sc[:, f, hs], pb_ap, sc_cv[:, m, hs], ALU.mult)
                    done_block(bi)
                outproj(wo, hf, md, lambda k, t2: yb_sc[:, k, t2 * TT:(t2 + 1) * TT])
            return run

        def ffn_phase(li, hf, md, ada_li=None, next_norm=None):
            l = layer_ids[li]
            pb = l * PL
            w_up = dr["ffn_w_up"][li]
            w_dn = dr["ffn_w_down"][li]
            seq = []
            ada = ada_parts(ada_li[0]) if ada_li is not None else None
            for i in range(6):
                ncols = 512 if i < 5 else 256
                aj = ada_li[1] + 2 * i if (ada and i < 3) else None
                seq.append(("v", i, ncols, wblock(w_up, 0, D, DFF + 512 * i, ncols), aj, ada[0](aj) if aj is not None else None))
                aj2 = ada_li[1] + 2 * i + 1 if (ada and i < 3) else None
                seq.append(("g", i, ncols, wblock(w_up, 0, D, 512 * i, ncols), aj2, ada[0](aj2) if aj2 is not None else None))
            dseq = []
            for cg in range(2):
                for kb in range(3):
                    nk = 8 if kb < 2 else 6
                    dseq.append((cg, kb, nk, wblock(w_dn, kb * 1024, nk * 128, cg * 512, 512)))

            def run():
                for si, (kind, i, ncols, bi, aj, abi) in enumerate(seq):
                    slot = use_block(bi)
                    Ub = U[0] if kind == "v" else U[1]
                    for m in range(ncols // 128):
                        jj = 4 * i + m
                        q = jj + (22 if kind == "v" else 0)
                        halo_in(Ub[:, m, 0:2], halo_u[:, q, :], hf)
                        w0, w1, w2 = [pcol(pb + P_FCW + t * NUP + q) for t in range(3)]
                        bc = pcol(pb + P_FCB + q)
                        accs = []
                        for t2 in range(2):
                            hs = slice(t2 * TT, (t2 + 1) * TT)
                            pb_ap = next_bank()
                            S.group(pb_ap, wchunks(slot, m), hcols(hs))
                            S.act(Ub[:, m, 2 + t2 * TT:2 + (t2 + 1) * TT], pb_ap, AF.Copy)
                            acc = (accv if kind == "v" else accg)[:, m, hs]
                            if kind == "v":
                                S.act(acc, pb_ap, AF.Identity, scale=w2, bias=bc)
                            accs.append(acc)
                        if kind == "g":
                            for t2 in range(2):
                                o = t2 * TT
                                S.ts(accs[t2], Ub[:, m, o + 2:o + 2 + TT], w2, bc, ALU.mult, ALU.add)
                        for t2 in range(2):
                            o = t2 * TT
                            S.stt(accs[t2], Ub[:, m, o + 1:o + 1 + TT], w1, accs[t2], ALU.mult, ALU.add)
                        for t2 in range(2):
                            o = t2 * TT
                            S.stt(accs[t2], Ub[:, m, o:o + TT], w0, accs[t2], ALU.mult, ALU.add)
                        S.copy_pool(halo_u[:, q, :], Ub[:, m, 1024:1026])
                        if kind == "g":
                            for t2 in range(2):
                                S.act(accs[t2], accs[t2], AF.Silu)
                            for t2 in range(2):
                                hs = slice(t2 * TT, (t2 + 1) * TT)
                                S.tt_pool(actv[:, jj, hs], accs[t2], accv[:, m, hs], ALU.mult)
                    done_block(bi)
                    if abi is not None:
                        ada[1](aj, abi)
                if ada and ada_li[2]:
                    ada[2]()
                gate = md[:, 40:48]
                side = norm_steps(*next_norm) if next_norm is not None else []
                for di, (cg, kb, nk, bi) in enumerate(dseq):
                    slot = use_block(bi)
                    for m in range(4):
                        mc = 4 * cg + m
                        for t2 in range(2):
                            hs = slice(t2 * TT, (t2 + 1) * TT)
                            pb_ap = next_bank()
                            S.group(pb_ap, wchunks(slot, m, nk), [actv[:, kb * 8 + k, hs] for k in range(nk)])
                            xupdate(pb_ap, mc, hf, t2, gate)
                    done_block(bi)
                    if di < len(side):
                        side[di]()
            return run

        def mlstm_phase(li, hf, md):
            l = layer_ids[li]
            j = ml_layers.index(l)
            pb = l * PL
            w_in = dr["ml_w_in"][j]
            b_g = wblock(w_in, 0, D, 3072, 8)
            b_q = wblock(w_in, 0, D, 0, 512)
            b_k = wblock(w_in, 0, D, 512, 512)
            b_v = [wblock(w_in, 0, D, 1024 + 512 * i, 512) for i in range(2)]
            b_o = [wblock(w_in, 0, D, 2048 + 512 * i, 512) for i in range(2)]
            wo = [wblock(dr["ml_w_out"][j], 0, D, 512 * cg, 512) for cg in range(2)]

            def run():
                if hf == 0:
                    S.memset(C32[:, :, :], 0.0)
                    S.memset(Cbf[:, :, :], 0.0)
                S.memset(kvv[:, :, :, DV:VA], 1.0, writes=[kv[:, :, 512:]])
                slot = use_block(b_g)
                pg = ps[:, 4, 384:448]
                for c8 in range(8):
                    S.group(pg[:, c8 * 8:(c8 + 1) * 8], hcols(slice(c8 * 128, (c8 + 1) * 128)),
                            [slot[:, k, 0:8] for k in range(NCH)])
                done_block(b_g)
                S.tt(G1, pg, params[:, pb + P_MIX + 8:pb + P_MIX + 72], ALU.add)
                S.act(TH, G1, AF.Tanh, scale=1.0 / 15.0)
                th3 = TH.rearrange("p (c g) -> p c g", c=8)
                S.ts(IGt, th3[:, :, 0:4], 15.0, None, ALU.mult)
                S.act(E4, th3[:, :, 4:8], AF.Exp, scale=-15.0)
                S.act(Lp, E4, AF.Ln, bias=one_col, scale=1.0)
                for which, bi in (("q", b_q), ("k", b_k)):
                    slot = use_block(bi)
                    for m in range(HEADS):
                        for t2 in range(2):
                            hs = slice(t2 * TT, (t2 + 1) * TT)
                            pb_ap = next_bank()
                            S.group(pb_ap, wchunks(slot, m), hcols(hs))
                            d_ = yb_ml[:, t2 * 4:(t2 + 1) * 4, (m if which == "q" else 4 + m), :]
                            src = pb_ap.rearrange("p (a b) -> p a b", a=4)
                            S.act(d_, src, AF.Copy, scale=(DK ** -0.5 if which == "q" else 1.0))
                    if which == "k":
                        for c8 in range(8):
                            pb_ap = next_bank()
                            S.group(pb_ap, hcols(slice(c8 * 128, (c8 + 1) * 128)), [slot[:, k, 0:512] for k in range(NCH)])
                            S.act(kv[:, c8, 0:512], pb_ap, AF.Copy)
                    done_block(bi)
                for i, bi in enumerate(b_v):
                    slot = use_block(bi)
                    for c8 in range(8):
                        pb_ap = next_bank()
                        S.group(pb_ap, hcols(slice(c8 * 128, (c8 + 1) * 128)), [slot[:, k, 0:512] for k in range(NCH)])
                        S.act(kvv[:, c8, 2 * i:2 * i + 2, 0:DV], pb_ap.rearrange("p (a b) -> p a b", a=2), AF.Copy)
                    done_block(bi)
                pbn = ps[:, 4, 0:8]

                def stage_a(c8):
                    par = c8 % 2
                    x16 = X16[:, par, :]
                    ex = EX[:, c8, :]
                    S.mm(pbn[:, 0:4], tri_f, Lp[:, c8, :], True, True, True)
                    S.mm(pbn[:, 4:8], ones_f, Lp[:, c8, :], True, True, True)
                    S.tt(x16[:, 0:4], IGt[:, c8, :], pbn[:, 0:4], ALU.add)
                    S.copy(x16[:, 8:12], pbn[:, 0:4])
                    S.ts(x16[:, 12:16], pbn[:, 4:8], -1.0, None, ALU.mult)
                    S.tt(x16[:, 4:8], x16[:, 0:4], pbn[:, 4:8], ALU.subtract)
                    S.act(ex, x16, AF.Exp)
                    pS = ps[:, 5 if par == 0 else 7, :].rearrange("p (h t) -> p h t", h=HEADS)
                    for h in range(HEADS):
                        S.mm(pS[:, h, :], yb_ml[:, c8, 4 + h, :], yb_ml[:, c8, h, :], True, True, True)
                    for h in range(HEADS):
                        S.stt(AT[:, par, h, :], pS[:, h, :], ex[:, h:h + 1], tri_bf[:, :], ALU.mult, ALU.mult)
                    for h in range(HEADS):
                        S.ts(KW[:, par, h, :], kv[:, c8, h * 128:(h + 1) * 128], ex[:, 4 + h:5 + h], None, ALU.mult)
                    for h in range(HEADS):
                        pn_ = ps[:, h, 0:VA]
                        S.mm(pn_, AT[:, par, h, :], kvv[:, c8, h, :], True, False, False)
                        S.mm(pn_, yb_ml[:, c8, h, :], Cbf[:, h, :], False, True, True)
                    for h in range(HEADS):
                        S.act(NUM[:, c8, h, :], ps[:, h, 0:VA], AF.Copy)
                    for h in range(HEADS):
                        S.mm(ps[:, h, 0:VA], KW[:, par, h, :], kvv[:, c8, h, :], True, True, True)
                    for h in range(HEADS):
                        S.stt(C32[:, h, :], C32[:, h, :], ex[:, 12 + h:13 + h], ps[:, h, 0:VA], ALU.mult, ALU.add)
                    for h in range(HEADS):
                        S.act(Cbf[:, h, :], C32[:, h, :], AF.Copy)
                    for h in range(HEADS):
                        nv = NUM[:, c8, h, 0:DV]
                        if h < 2:
                            S.act(JUNK[:, h, :], nv, AF.Square, accum_out=SSQ[:, c8, h:h + 1])
                        else:
                            S.op("dve", lambda e, nv=nv, jk=JUNK[:, h, :], acc=SSQ[:, c8, h:h + 1]: e.scalar_tensor_tensor(
                                out=jk, in0=nv, scalar=1.0, in1=nv, op0=ALU.mult, op1=ALU.mult, accum_out=acc),
                                [nv], [JUNK[:, h, :], SSQ[:, c8, h:h + 1]])

                for c8 in range(8):
                    stage_a(c8)

                S.act(DN, NUM[:, :, :, DV], AF.Abs)
                S.tt(DN, DN, EX[:, :, 8:12], ALU.max)
                S.recip(DN, DN)
                S.tt(SSQ, SSQ, DN, ALU.mult)
                S.tt(SSQ, SSQ, DN, ALU.mult)
                S.act(SQ, SSQ, AF.Ln, scale=1.0 / DV, bias=eps_col)
                S.act(SQ, SQ, AF.Exp, scale=-0.5)
                S.tt(RR, SQ, DN, ALU.mult)
                for c8 in range(8):
                    hb = c8 % 3
                    for h in range(HEADS):
                        S.ts(HH[:, hb, h, :], NUM[:, c8, h, 0:DV], RR[:, c8, h:h + 1], None, ALU.mult)
                    pTb = ps[:, (5, 6, 7)[c8 % 3], :].bitcast(BF16).rearrange("p (h j t) -> p h j t", h=HEADS, j=2)
                    for h in range(HEADS):
                        for jj in range(2):
                            S.transpose(pTb[:, h, jj, :], HH[:, hb, h, jj * 128:(jj + 1) * 128], ident_bf[:, :])
                    on_act = (c8 % 3 == 2)
                    for h in range(HEADS):
                        for jj in range(2):
                            gcol = pcol(pb + P_MIX + 2 * h + jj)
                            if on_act:
                                S.act(yb_ml[:, c8, 2 * h + jj, :], pTb[:, h, jj, :], AF.Copy, scale=gcol)
                            else:
                                S.ts(yb_ml[:, c8, 2 * h + jj, :], pTb[:, h, jj, :], gcol, None, ALU.mult)
                for i, bi in enumerate(b_o):
                    slot = use_block(bi)
                    for m in range(4):
                        f = 4 * i + m
                        for t2 in range(2):
                            hs = slice(t2 * TT, (t2 + 1) * TT)
                            pb_ap = next_bank()
                            S.group(pb_ap, wchunks(slot, m), hcols(hs))
                            so = so_v[:, t2, :]
                            S.act(so, pb_ap, AF.Sigmoid)
                            d_ = yb_ml[:, t2 * 4:(t2 + 1) * 4, f, :]
                            S.tt(d_, d_, so.rearrange("p (a b) -> p a b", a=4), ALU.mult)
                    done_block(bi)
                outproj(wo, hf, md, lambda k, t2: yb_ml[:, t2 * 4:(t2 + 1) * 4, k, :])
            return run

        eps_col = sb("eps_col", [128, 1], F32)
        one_col = sb("one_col", [128, 1], F32)
        S.memset(eps_col[:, :], EPS)
        S.memset(one_col[:, :], 1.0)
        eps_col = eps_col[:, :]
        one_col = one_col[:, :]

        sequence = [ada_phase(0)]
        for li, l in enumerate(layer_ids):
            md = mods[:, l % 2, :]
            for hf in range(2):
                if li == 0 and hf == 0:
                    sequence.append(("norm", hf, md[:, 8:16], md[:, 0:8]))
                if l % 2 == 0:
                    sequence.append(shortconv_phase(li, hf, md))
                else:
                    sequence.append(mlstm_phase(li, hf, md))
                if hf == 0:
                    nn = (1, md[:, 8:16], md[:, 0:8])
                elif li + 1 < nl:
                    md2 = mods[:, layer_ids[li + 1] % 2, :]
                    nn = (0, md2[:, 8:16], md2[:, 0:8])
                else:
                    nn = None
                sequence.append(ffn_phase(li, hf, md, ada_li=((li + 1, 6 * hf, hf == 1) if li + 1 < nl else None), next_norm=nn))
        for item in sequence:
            if isinstance(item, tuple):
                norm_phase(item[1], item[2], item[3])
            else:
                item()

        yTv = yT.rearrange("(c p) t -> p c t", p=128)
        if final_norm:
            fg = params[:, P_FNG:P_FNG + 8]
            for tt in range(4):
                tok = slice(tt * TT, (tt + 1) * TT)
                rsv = rs_v[:, tt % 2, :]
                rstd_tile(rsv, tok, lambda c: h_sb[:, c, 0:TT])
                for c in range(NCH):
                    xs = x_sb[:, c, tok]
                    S.stt(xs, xs, fg[:, c:c + 1], rsv, ALU.mult, ALU.mult)
        tok_last = None
        for c in range(NCH):
            tok_last = S.dma("sp", yTv[:, c, :], x_sb[:, c, :], st_sem)
        S.wait_only("sp", [tok_last])
        S.emit()
        nc._sched_stats = S.stats
    return nc


def _consts():
    c = np.zeros((128, 512), np.float32)
    c[:, 0:128] = np.eye(128, dtype=np.float32)
    j = np.arange(128)[:, None]
    t = np.arange(128)[None, :]
    c[:, 128:256] = (j <= t).astype(np.float32)
    c[:, 256:384] = np.where(j <= t, 0.0, 30000.0)
    c[:, 384:512] = 1.0
    return c


def _chunked(v):
    v = np.asarray(v, np.float32)
    return np.ascontiguousarray(v.reshape(-1, 128).T)


def _params_for_core(b, inp):
    P = np.zeros((128, NP_COLS), np.float32)
    for l in range(DEPTH):
        pb = l * PL
        j = l // 2
        P[:, pb + P_ADAB:pb + P_ADAB + 48] = _chunked(inp["ada_b"][l])
        P[:, pb + P_NMG:pb + P_NMG + 8] = _chunked(inp["norm_mix_g"][l])
        P[:, pb + P_NFG:pb + P_NFG + 8] = _chunked(inp["norm_ffn_g"][l])
        if l % 2 == 0:
            for t in range(3):
                P[:, pb + P_MIX + t * 8:pb + P_MIX + (t + 1) * 8] = _chunked(inp["sc_conv_w"][j][t])
        else:
            P[:, pb + P_MIX:pb + P_MIX + 8] = _chunked(inp["ml_norm_g"][j])
            bif = np.concatenate([np.asarray(inp["ml_b_i"][j], np.float32), np.asarray(inp["ml_b_f"][j], np.float32)])
            P[:, pb + P_MIX + 8:pb + P_MIX + 72] = np.tile(bif, 8)[None, :]
        for t in range(3):
            P[:, pb + P_FCW + t * NUP:pb + P_FCW + (t + 1) * NUP] = _chunked(inp["ffn_conv_w"][l][t])
        P[:, pb + P_FCB:pb + P_FCB + NUP] = _chunked(inp["ffn_conv_b"][l])
    P[:, P_C:P_C + 8] = _chunked(inp["c"][b])
    P[:, P_FNG:P_FNG + 8] = _chunked(inp["final_norm_g"])
    return P


_PROG_CACHE = {}


def _get_prog(layer_ids, final_norm):
    key = (tuple(layer_ids), final_norm)
    if key not in _PROG_CACHE:
        _PROG_CACHE[key] = build_program(list(layer_ids), final_norm)
    return _PROG_CACHE[key]


def _launch(layer_ids, final_norm, xT_list, inp, params_list, consts):
    nc = _get_prog(layer_ids, final_norm)
    l0, l1 = layer_ids[0], layer_ids[-1] + 1
    sc = [l // 2 for l in layer_ids if l % 2 == 0]
    ml = [l // 2 for l in layer_ids if l % 2 == 1]
    shared = {
        "consts": consts,
        "ada_w": np.ascontiguousarray(inp["ada_w"][l0:l1]),
        "ffn_w_up": np.ascontiguousarray(inp["ffn_w_up"][l0:l1]),
        "ffn_w_down": np.ascontiguousarray(inp["ffn_w_down"][l0:l1]),
    }
    if sc:
        shared["sc_w_in"] = np.ascontiguousarray(inp["sc_w_in"][sc[0]:sc[-1] + 1])
        shared["sc_w_out"] = np.ascontiguousarray(inp["sc_w_out"][sc[0]:sc[-1] + 1])
    if ml:
        shared["ml_w_in"] = np.ascontiguousarray(inp["ml_w_in"][ml[0]:ml[-1] + 1])
        shared["ml_w_out"] = np.ascontiguousarray(inp["ml_w_out"][ml[0]:ml[-1] + 1])
    in_maps = []
    for b in range(8):
        m = dict(shared)
        m["xT"] = xT_list[b]
        m["params"] = params_list[b]
        in_maps.append(m)
    res = run_bass_kernel_spmd(nc, in_maps, core_ids=list(range(8)))
    return [np.asarray(r["yT"]) for r in res.results]


LAUNCH_GROUPS = [[0, 1, 2, 3]]


def kernel(**inputs):
    inp = {k: np.asarray(v) for k, v in inputs.items()}
    x = inp["x"].astype(np.float32, copy=False)
    consts = _consts()
    params_list = [_params_for_core(b, inp) for b in range(8)]
    xT_list = [np.ascontiguousarray(x[b].T) for b in range(8)]
    for gi, grp in enumerate(LAUNCH_GROUPS):
        xT_list = _launch(grp, gi == len(LAUNCH_GROUPS) - 1, xT_list, inp, params_list, consts)
    out = np.stack([np.ascontiguousarray(xT_list[b].T) for b in range(8)], axis=0)
    return out.astype(np.float32, copy=False)
```
